# Optimizing a Trainium2 kernel written in Bass

```python
import math
import jax, jax.numpy as jnp
from jax import lax
import numpy as np

D_MODEL = 1024
BATCH = 16
SEQ = 2048
DEPTH = 2

ATT_HEADS = 8
ATT_HEAD_DIM = 64
ATT_WIDTH = ATT_HEADS * ATT_HEAD_DIM
Q_BLOCK = 128
SSM_GROUP = 16
SSM_WIDTH = D_MODEL // 2
SSM_GROUPS = SSM_WIDTH // SSM_GROUP
SSM_STATE = 64
DT_MIN = 1e-3
DT_MAX = 1e-1
D_FF = 256 * ((8 * D_MODEL // 3 + 255) // 256)
N_EXPERTS = 8
TOP_K = 2
N_DENSE = (DEPTH + 1) // 2
N_MOE = DEPTH // 2
N_IN = 3 * ATT_WIDTH + ATT_HEADS + SSM_WIDTH + 2 * D_MODEL
EPS = 1e-6

kernel_name = 'fox_s5_gated_hybrid_moe_block'


def _rmsnorm(x, g):
    xf = x.astype(jnp.float32)
    y = xf * lax.rsqrt(jnp.mean(xf * xf, axis=-1, keepdims=True) + EPS)
    return (y * g.astype(jnp.float32)).astype(x.dtype)


def _forgetting_attention(q, k, v, log_f):
    bsz, seq, heads, dh = q.shape
    nblk = seq // Q_BLOCK
    cum = jnp.cumsum(log_f, axis=1).transpose(0, 2, 1)
    kh = k.transpose(0, 2, 1, 3)
    vh = v.transpose(0, 2, 1, 3)
    q_blocks = q.transpose(0, 2, 1, 3).reshape(bsz, heads, nblk, Q_BLOCK, dh).transpose(2, 0, 1, 3, 4)
    c_blocks = cum.reshape(bsz, heads, nblk, Q_BLOCK).transpose(2, 0, 1, 3)
    starts = jnp.arange(nblk, dtype=jnp.int32) * Q_BLOCK
    key_pos = jnp.arange(seq, dtype=jnp.int32)
    scale = dh ** -0.5

    def one_block(args):
        qb, cb, s0 = args
        logits = jnp.einsum('bhqd,bhkd->bhqk', qb, kh, preferred_element_type=jnp.float32) * scale
        logits = logits + cb[..., None] - cum[:, :, None, :]
        q_pos = s0 + jnp.arange(Q_BLOCK, dtype=jnp.int32)
        causal = q_pos[:, None] >= key_pos[None, :]
        logits = jnp.where(causal, logits, -jnp.inf)
        p = jax.nn.softmax(logits, axis=-1)
        return jnp.einsum('bhqk,bhkd->bhqd', p.astype(vh.dtype), vh)

    out = lax.map(one_block, (q_blocks, c_blocks, starts))
    return out.transpose(1, 0, 3, 2, 4).reshape(bsz, seq, heads * dh)


def _s5(u, lam_re, lam_im, log_dt, b_re, b_im, c_re, c_im, d_skip, w_glu, b_glu):
    bsz, seq, _ = u.shape
    f32 = jnp.float32
    uf = u.astype(f32).reshape(bsz, seq, SSM_GROUPS, SSM_GROUP)
    lr, li = lam_re.astype(f32), lam_im.astype(f32)
    dt = jnp.exp(log_dt.astype(f32))[:, None]
    mag = jnp.exp(lr * dt)
    ang = li * dt
    a_re, a_im = mag * jnp.cos(ang), mag * jnp.sin(ang)
    n_re, n_im = a_re - 1.0, a_im
    den = lr * lr + li * li
    k_re = (n_re * lr + n_im * li) / den
    k_im = (n_im * lr - n_re * li) / den
    br, bi = b_re.astype(f32), b_im.astype(f32)
    bb_re = k_re[..., None] * br - k_im[..., None] * bi
    bb_im = k_re[..., None] * bi + k_im[..., None] * br
    bu_re = jnp.einsum('blgh,gph->blgp', uf, bb_re)
    bu_im = jnp.einsum('blgh,gph->blgp', uf, bb_im)
    a_re_t = jnp.broadcast_to(a_re, (1, seq, SSM_GROUPS, SSM_STATE))
    a_im_t = jnp.broadcast_to(a_im, (1, seq, SSM_GROUPS, SSM_STATE))

    def combine(e1, e2):
        a1r, a1i, b1r, b1i = e1
        a2r, a2i, b2r, b2i = e2
        return (a2r * a1r - a2i * a1i,
                a2r * a1i + a2i * a1r,
                a2r * b1r - a2i * b1i + b2r,
                a2r * b1i + a2i * b1r + b2i)

    _, _, s_re, s_im = lax.associative_scan(combine, (a_re_t, a_im_t, bu_re, bu_im), axis=1)
    y = (jnp.einsum('blgp,ghp->blgh', s_re, c_re.astype(f32))
         - jnp.einsum('blgp,ghp->blgh', s_im, c_im.astype(f32))
         + d_skip.astype(f32) * uf)
    y = jax.nn.gelu(y.reshape(bsz, seq, SSM_WIDTH))
    y = y * jax.nn.sigmoid(y @ w_glu.astype(f32) + b_glu.astype(f32))
    return y.astype(u.dtype)


def _swiglu(t, w1, w3, w2):
    return (jax.nn.silu(t @ w1) * (t @ w3)) @ w2


def _moe(h, w_router, b_router, w1, w3, w2):
    bsz, seq, d = h.shape
    t = h.reshape(-1, d)
    logits = (t @ w_router + b_router).astype(jnp.float32)
    top_val, top_idx = lax.top_k(logits, TOP_K)
    top_w = jax.nn.softmax(top_val, axis=-1)
    gates = jnp.sum(jax.nn.one_hot(top_idx, N_EXPERTS, dtype=jnp.float32) * top_w[..., None], axis=1)
    out = jnp.zeros_like(t)
    for e in range(N_EXPERTS):
        out = out + gates[:, e:e + 1].astype(t.dtype) * _swiglu(t, w1[e], w3[e], w2[e])
    return out.reshape(bsz, seq, d)


def setup_inputs(seed: int = 0) -> dict:
    key = jax.random.key(seed)
    ks = jax.random.split(key, 32)
    f32 = jnp.float32
    D = D_MODEL
    G, P, Hc = SSM_GROUPS, SSM_STATE, SSM_GROUP

    def nrm(k, shape, scale):
        return jax.random.normal(k, shape, f32) * scale

    n_idx = jnp.arange(P, dtype=f32)
    return {
        'x': nrm(ks[0], (BATCH, SEQ, D), 1.0),
        'c': nrm(ks[1], (BATCH, D), 1.0),
        'w_ada': nrm(ks[2], (DEPTH, D, 6 * D), D ** -0.5),
        'b_ada': nrm(ks[3], (DEPTH, 6 * D), 0.02),
        'g_mix': 1.0 + nrm(ks[4], (DEPTH, D), 0.05),
        'w_in': nrm(ks[5], (DEPTH, D, N_IN), D ** -0.5),
        'b_forget': 2.0 + nrm(ks[6], (DEPTH, ATT_HEADS), 0.5),
        'b_gate': nrm(ks[7], (DEPTH, 2 * D), 0.02),
        'lam_re': -0.5 + nrm(ks[8], (DEPTH, G, P), 0.01),
        'lam_im': math.pi * n_idx + nrm(ks[9], (DEPTH, G, P), 0.01),
        'log_dt': jax.random.uniform(ks[10], (DEPTH, G), f32, math.log(DT_MIN), math.log(DT_MAX)),
        'b_re': nrm(ks[11], (DEPTH, G, P, Hc), (2 * Hc) ** -0.5),
        'b_im': nrm(ks[12], (DEPTH, G, P, Hc), (2 * Hc) ** -0.5),
        'c_re': nrm(ks[13], (DEPTH, G, Hc, P), P ** -0.5),
        'c_im': nrm(ks[14], (DEPTH, G, Hc, P), P ** -0.5),
        'd_skip': nrm(ks[15], (DEPTH, G, Hc), 1.0),
        'w_glu': nrm(ks[16], (DEPTH, SSM_WIDTH, SSM_WIDTH), SSM_WIDTH ** -0.5),
        'b_glu': nrm(ks[17], (DEPTH, SSM_WIDTH), 0.02),
        'w_proj_att': nrm(ks[18], (DEPTH, ATT_WIDTH, D), ATT_WIDTH ** -0.5),
        'w_proj_ssm': nrm(ks[19], (DEPTH, SSM_WIDTH, D), SSM_WIDTH ** -0.5),
        'w_out': nrm(ks[20], (DEPTH, D, D), D ** -0.5),
        'g_ffn': 1.0 + nrm(ks[21], (DEPTH, D), 0.05),
        'w1_dense': nrm(ks[22], (N_DENSE, D, D_FF), D ** -0.5),
        'w3_dense': nrm(ks[23], (N_DENSE, D, D_FF), D ** -0.5),
        'w2_dense': nrm(ks[24], (N_DENSE, D_FF, D), D_FF ** -0.5),
        'w_router': nrm(ks[25], (N_MOE, D, N_EXPERTS), D ** -0.5),
        'b_router': nrm(ks[26], (N_MOE, N_EXPERTS), 0.01),
        'w1_moe': nrm(ks[27], (N_MOE, N_EXPERTS, D, D_FF), D ** -0.5),
        'w3_moe': nrm(ks[28], (N_MOE, N_EXPERTS, D, D_FF), D ** -0.5),
        'w2_moe': nrm(ks[29], (N_MOE, N_EXPERTS, D_FF, D), D_FF ** -0.5),
        'g_final': 1.0 + nrm(ks[30], (D,), 0.05),
    }


def reference(x, c, w_ada, b_ada, g_mix, w_in, b_forget, b_gate, lam_re, lam_im, log_dt,
              b_re, b_im, c_re, c_im, d_skip, w_glu, b_glu, w_proj_att, w_proj_ssm, w_out,
              g_ffn, w1_dense, w3_dense, w2_dense, w_router, b_router, w1_moe, w3_moe, w2_moe,
              g_final):
    bsz, seq, d = x.shape
    cond = jax.nn.silu(c)
    splits = [ATT_WIDTH, 2 * ATT_WIDTH, 3 * ATT_WIDTH, 3 * ATT_WIDTH + ATT_HEADS,
              3 * ATT_WIDTH + ATT_HEADS + SSM_WIDTH]
    for l in range(DEPTH):
        ada = (cond @ w_ada[l] + b_ada[l])[:, None, :]
        sh1, sc1, gt1, sh2, sc2, gt2 = jnp.split(ada, 6, axis=-1)

        h = _rmsnorm(x, g_mix[l]) * (1.0 + sc1) + sh1
        z = h @ w_in[l]
        q, k, v, fg, u, gates = jnp.split(z, splits, axis=-1)
        log_f = jax.nn.log_sigmoid((fg + b_forget[l]).astype(jnp.float32))
        hs = (bsz, seq, ATT_HEADS, ATT_HEAD_DIM)
        y_att = _forgetting_attention(q.reshape(hs), k.reshape(hs), v.reshape(hs), log_f) @ w_proj_att[l]
        y_ssm = _s5(u, lam_re[l], lam_im[l], log_dt[l], b_re[l], b_im[l], c_re[l], c_im[l],
                    d_skip[l], w_glu[l], b_glu[l]) @ w_proj_ssm[l]
        g_att, g_ssm = jnp.split(jax.nn.sigmoid(gates + b_gate[l]), 2, axis=-1)
        mixed = (g_att * y_att + g_ssm * y_ssm) @ w_out[l]
        x = x + gt1 * mixed

        h2 = _rmsnorm(x, g_ffn[l]) * (1.0 + sc2) + sh2
        if l % 2 == 0:
            m = l // 2
            f = _swiglu(h2, w1_dense[m], w3_dense[m], w2_dense[m])
        else:
            m = l // 2
            f = _moe(h2, w_router[m], b_router[m], w1_moe[m], w3_moe[m], w2_moe[m])
        x = x + gt2 * f
    return _rmsnorm(x, g_final)
```

```python
import math
from contextlib import ExitStack
import numpy as np
import concourse.bass as bass
import concourse.mybir as mybir
from concourse.bass_utils import run_bass_kernel_spmd

F32 = mybir.dt.float32
BF16 = mybir.dt.bfloat16
I32 = mybir.dt.int32
AF = mybir.ActivationFunctionType
ALU = mybir.AluOpType
AX = mybir.AxisListType

NCORES = 8
D = 1024
T = 2048
NT = 16
DEPTH = 2
H = 8
DFF = 2816
NFT = 22
NE = 8
N_IN = 4104
EPS = 1e-6
NEGBIG = -30000.0
TWO_PI = 2.0 * math.pi
GELU_C = 1.5957691216057308
FGROUPS = [(0, 6), (6, 6), (12, 5), (17, 5)]

ENGINES = ("pe", "act", "dve", "pool", "sp")

IN_SPECS = {
    "x": [2, T, D], "c": [2, D], "w_ada": [2, D, 6 * D], "b_ada": [2, 6 * D], "g_mix": [2, D],
    "w_in": [2, D, N_IN], "b_forget": [2, 8], "b_gate": [2, 2 * D], "lam_re": [2, 32, 64],
    "lam_im": [2, 32, 64], "log_dt": [2, 32], "b_re": [2, 32, 64, 16], "b_im": [2, 32, 64, 16],
    "c_re": [2, 32, 16, 64], "c_im": [2, 32, 16, 64], "d_skip": [2, 32, 16], "w_glu": [2, 512, 512],
    "b_glu": [2, 512], "w_proj_att": [2, 512, D], "w_proj_ssm": [2, 512, D], "w_out": [2, D, D],
    "g_ffn": [2, D], "w1_dense": [1, D, DFF], "w3_dense": [1, D, DFF], "w2_dense": [1, DFF, D],
    "w_router": [1, D, 8], "b_router": [1, 8], "w1_moe": [1, 8, D, DFF], "w3_moe": [1, 8, D, DFF],
    "w2_moe": [1, 8, DFF, D], "g_final": [D],
}


class Buf:
    __slots__ = ("name", "w", "r")

    def __init__(self, name):
        self.name = name
        self.w = None
        self.r = {}


class Sched:
    def __init__(self, nc, stack):
        self.nc = nc
        self.stack = stack
        self.ops = {e: [] for e in ENGINES}
        self.cnt = {}
        self.known = {e: {} for e in ENGINES}
        self.sems = {}
        self.pe_pending = []
        self.cur = None

    def sem(self, key):
        if key not in self.sems:
            self.sems[key] = self.stack.enter_context(self.nc.semaphore(str(key)))
            self.cnt[key] = 0
        return self.sems[key]

    def _collect(self, eng, reads, writes):
        waits = {}
        for b in reads:
            if b.w is not None and waits.get(b.w[0], 0) < b.w[1]:
                waits[b.w[0]] = b.w[1]
        for b in writes:
            if b.w is not None and waits.get(b.w[0], 0) < b.w[1]:
                waits[b.w[0]] = b.w[1]
            for k, v in b.r.items():
                if waits.get(k, 0) < v:
                    waits[k] = v
        out = []
        kn = self.known[eng]
        for k, v in waits.items():
            if kn.get(k, 0) < v:
                kn[k] = v
                out.append((k, v))
        return out

    @staticmethod
    def _mark(ev, reads, writes):
        for b in writes:
            b.w = ev
            b.r = {}
        for b in reads:
            if b.r.get(ev[0], 0) < ev[1]:
                b.r[ev[0]] = ev[1]

    def op(self, eng, method, kw, reads=(), writes=()):
        waits = self._collect(eng, reads, writes)
        self.sem(eng)
        self.cnt[eng] += 1
        ev = (eng, self.cnt[eng])
        self._emit(eng, (waits, method, kw, eng, 1))
        self._mark(ev, reads, writes)
        return ev

    def _emit(self, eng, rec):
        if self.cur is None:
            self.ops[eng].append(rec)
        else:
            self.cur["ops"][eng].append(rec)
            if rec[3] is not None:
                d = self.cur["incs"][eng]
                d[rec[3]] = d.get(rec[3], 0) + rec[4]

    def cond_begin(self, flag_ap, reads):
        assert self.cur is None and not self.pe_pending
        snap = {e: dict(self.known[e]) for e in ENGINES}
        waits, after = {}, {}
        for e in ENGINES:
            waits[e] = self._collect(e, reads, ())
            after[e] = dict(self.known[e])
        self.cur = {"flag": flag_ap, "ops": {e: [] for e in ENGINES}, "incs": {e: {} for e in ENGINES},
                    "waits": waits, "snap": snap, "after": after, "base": dict(self.cnt)}

    def cond_end(self):
        assert not self.pe_pending
        blk = self.cur
        self.cur = None
        for e in ENGINES:
            if blk["ops"][e]:
                base = {k: blk["base"].get(k, 0) for k in blk["incs"][e]}
                self.ops[e].append(("COND", blk["flag"], blk["waits"][e], blk["ops"][e], blk["incs"][e], base))
                self.known[e] = blk["after"][e]
            else:
                self.known[e] = blk["snap"][e]

    def pe(self, method, kw, reads=(), writes=(), last=True):
        if not last:
            waits = self._collect("pe", reads, writes)
            self._emit("pe", (waits, method, kw, None, 0))
            self.pe_pending.append((list(reads), list(writes)))
            return None
        ev = self.op("pe", method, kw, reads, writes)
        for (r, w) in self.pe_pending:
            self._mark(ev, r, w)
        self.pe_pending = []
        return ev

    def dma(self, queue, kw, reads=(), writes=(), semkey=None, method="dma_start"):
        waits = self._collect(queue, reads, writes)
        if semkey is None:
            semkey = "dma_" + (writes[0].name if writes else reads[0].name)
        self.sem(semkey)
        self.cnt[semkey] += 16
        ev = (semkey, self.cnt[semkey])
        self._emit(queue, (waits, method, kw, semkey, 16))
        self._mark(ev, reads, writes)
        return ev

    def barrier(self, exclude=()):
        assert not self.pe_pending
        for e in ENGINES:
            waits = []
            kn = self.known[e]
            for k, v in self.cnt.items():
                if k in exclude:
                    continue
                if kn.get(k, 0) < v:
                    kn[k] = v
                    waits.append((k, v))
            if waits:
                self.ops[e].append((waits, None, None, None, 0))

    def run(self):
        nc = self.nc
        ops = self.ops
        sems = self.sems

        regs = {}

        def replay(e, lst):
            for rec in lst:
                if rec[0] == "COND":
                    _, flag_ap, waits, inner, incs, base = rec
                    for (k, v) in waits:
                        e.wait_ge(sems[k], v)
                    regs["n"] = regs.get("n", 0) + 1
                    reg = e.alloc_register(f"condreg{regs['n']}")
                    e.reg_load(reg, flag_ap)
                    cv = e.snap(reg, donate=True)
                    with e.If(cv > 0):
                        replay(e, inner)
                    with e.Else():
                        for k, n in incs.items():
                            if base[k] > 0:
                                e.wait_ge(sems[k], base[k])
                            e.sem_inc(sems[k], n)
                    try:
                        nc.free_register(reg)
                    except Exception:
                        pass
                    continue
                (waits, method, kw, key, n) = rec
                for (k, v) in waits:
                    e.wait_ge(sems[k], v)
                if method is None:
                    continue
                ins = getattr(e, method)(**kw)
                if key is not None:
                    ins.then_inc(sems[key], n)

        with nc.Block() as block:
            @block.tensor
            def _(e):
                replay(e, ops["pe"])

            @block.scalar
            def _(e):
                replay(e, ops["act"])

            @block.vector
            def _(e):
                replay(e, ops["dve"])

            @block.gpsimd
            def _(e):
                replay(e, ops["pool"])

            @block.sync
            def _(e):
                replay(e, ops["sp"])


def build(debug=False, stop=None, n_seq=2, n_layers=DEPTH, do_setup=True, sparse=True):
    nc = bass.Bass("TRN2", target_bir_lowering=False)
    Dm = {k: nc.dram_tensor(k, shp, F32, kind="ExternalInput").ap() for k, shp in IN_SPECS.items()}
    out_d = nc.dram_tensor("out", [2, T, D], F32, kind="ExternalOutput").ap()
    WSSM = nc.dram_tensor("scr_wssm", [2, 128, 32, 4, 128], BF16, kind="Internal").ap()
    TAB = nc.dram_tensor("scr_tab", [2, 128, 32, 2, 256], F32, kind="Internal").ap()
    RHOD = nc.dram_tensor("scr_rho", [2, 128, 32], F32, kind="Internal").ap()
    HS = nc.dram_tensor("scr_hs", [NE * T, D], BF16, kind="Internal").ap()
    W1B = nc.dram_tensor("scr_w1b", [NE, D, DFF], BF16, kind="Internal").ap()
    W3B = nc.dram_tensor("scr_w3b", [NE, D, DFF], BF16, kind="Internal").ap()
    W2B = nc.dram_tensor("scr_w2b", [NE, DFF, D], BF16, kind="Internal").ap()
    RS = nc.dram_tensor("scr_rs", [NE * T, D], F32, kind="Internal").ap()
    dbg_out = {}

    st = ExitStack()
    S = Sched(nc, st)
    sb = lambda name, shape, dt: st.enter_context(nc.sbuf_tensor(name, shape, dt))

    def DVE(method, r, w, **kw):
        return S.op("dve", method, kw, r, w)

    def ACT(method, r, w, **kw):
        return S.op("act", method, kw, r, w)

    def POOL(method, r, w, **kw):
        return S.op("pool", method, kw, r, w)

    def PE(method, r, w, last=True, **kw):
        return S.pe(method, kw, r, w, last=last)

    def DMA(queue, r, w, semkey=None, **kw):
        return S.dma(queue, kw, r, w, semkey=semkey)

    X = sb("X", [128, NT, D], F32)
    bX = [Buf(f"X{t}") for t in range(NT)]
    ADA = sb("ADA", [128, 3, D], F32)
    bADA = Buf("ADA")
    ident_bf = sb("ident_bf", [128, 128], BF16)
    ident_f = sb("ident_f", [128, 128], F32)
    ones_bf = sb("ones_bf", [128, 64], BF16)
    neg8 = sb("neg8", [128, 128], BF16)
    maskneg = sb("maskneg", [128, 128], BF16)
    ones_f = sb("ones_f", [128, 1], F32)
    jmask = sb("jmask", [128, 8], F32)
    ss = sb("ss", [128, NT], F32)
    rstd = sb("rstd", [128, NT], F32)
    rho = sb("rho", [128, 32], F32)
    bCONST = Buf("const")
    bSS = Buf("ss")
    ARENA_BYTES = 131072
    arena = sb("arena", [128, ARENA_BYTES // 4], F32)
    pbank = [st.enter_context(nc.psum_tensor(f"pb{i}", [128, 512], F32)) for i in range(8)]
    bP = [Buf(f"pb{i}") for i in range(8)]
    Xflat = X[:].rearrange("p a b -> p (a b)")

    def mkview(flat, cap, off, shape, dt):
        size = 2 if dt == BF16 else 4
        n = 1
        for s_ in shape[1:]:
            n *= s_
        assert off % 4 == 0 and (n * size) % 4 == 0 and off + n * size <= cap, (off, shape, cap)
        v = flat[0:shape[0], off // 4:(off + n * size) // 4]
        if dt != F32:
            v = v.bitcast(dt)
        if len(shape) > 2:
            names = [f"f{i}" for i in range(len(shape) - 1)]
            kw = {nm: s_ for nm, s_ in zip(names[:-1], shape[1:-1])}
            v = v.rearrange(f"p ({' '.join(names)}) -> p {' '.join(names)}", **kw)
        return v

    def aview(off, shape, dt):
        return mkview(arena, ARENA_BYTES, off, shape, dt)

    def xview(off, shape, dt):
        return mkview(Xflat, NT * D * 4, off, shape, dt)

    def dbg(name, ap, shape, dt, reads):
        if not debug:
            return
        t = nc.dram_tensor("dbg_" + name, shape, dt, kind="ExternalOutput").ap()
        dbg_out[name] = t
        DMA("sp", reads, [], semkey="dbg", out=t, in_=ap)

    def diag_select(t, op, base=0, cm=1, step=-1, n=128):
        POOL("affine_select", [bCONST], [bCONST], out=t, in_=t, pattern=[[step, n]], compare_op=op, fill=0.0,
             base=base, channel_multiplier=cm)

    POOL("memset", [], [bCONST], ap=ident_bf[:], constant=1.0)
    diag_select(ident_bf[:], ALU.is_equal)
    POOL("memset", [], [bCONST], ap=ident_f[:], constant=1.0)
    diag_select(ident_f[:], ALU.is_equal)
    POOL("memset", [], [bCONST], ap=ones_bf[:], constant=1.0)
    POOL("memset", [], [bCONST], ap=neg8[:], constant=-8.0)
    POOL("memset", [], [bCONST], ap=ones_f[:], constant=1.0)
    POOL("memset", [], [bCONST], ap=maskneg[:], constant=NEGBIG)
    diag_select(maskneg[:], ALU.is_gt)
    POOL("memset", [], [bCONST], ap=jmask[:], constant=1.0)
    diag_select(jmask[:], ALU.is_ge, base=0, cm=1, step=-16, n=8)
    diag_select(jmask[:], ALU.is_ge, base=15, cm=-1, step=16, n=8)

    evac_rr = [0]

    def evac(out_ap, in_ap, reads, writes, eng=None):
        if eng is None:
            eng = "act" if evac_rr[0] % 2 == 0 else "dve"
            evac_rr[0] += 1
        if eng == "act":
            ACT("copy", reads, writes, out=out_ap, in_=in_ap)
        else:
            S.op(eng, "tensor_copy", dict(out=out_ap, in_=in_ap), reads, writes)

    def load_x(s):
        xv = Dm["x"][s].rearrange("(t p) d -> p t d", p=128)
        for g in range(4):
            DMA("sp", [], bX[4 * g:4 * g + 4], semkey=f"dma_X{g}", out=X[:, 4 * g:4 * g + 4, :],
                in_=xv[:, 4 * g:4 * g + 4, :])

    def compute_ada(s, l, half, off):
        condT = aview(off, [128, 8], F32)
        condB = aview(off + 64, [128, 8, 128], BF16)
        gtmp = aview(off + 64 + 2048, [128, D], F32)
        wsl = [aview(off + 64 + 2048 + 4096 + i * 8192, [128, 8, 512], BF16) for i in range(6)]
        b_cond, b_condB, b_g = Buf("cond"), Buf("condB"), Buf("gtmp")
        b_w = [Buf(f"adaw{i}") for i in range(6)]
        DMA("sp", [], [b_cond], semkey="dma_cond", out=condT, in_=Dm["c"][s].rearrange("(kt p) -> p kt", p=128),
            allow_slow_non_contiguous=True)
        ACT("activation", [b_cond], [b_cond], out=condT, in_=condT, func=AF.Silu)
        DVE("tensor_copy", [b_cond], [b_condB], out=condB, in_=condT.unsqueeze(2).to_broadcast([128, 8, 128]))
        gname = "g_mix" if half == 0 else "g_ffn"
        DMA("sp", [], [b_g], semkey="dma_gtmp", out=gtmp, in_=Dm[gname][l].partition_broadcast(128))
        c_base = half * 3 * D
        DMA("sp", [], [bADA], semkey="dma_ADA", out=ADA[:].rearrange("p a b -> p (a b)"),
            in_=Dm["b_ada"][l, c_base:c_base + 3 * D].partition_broadcast(128))
        for j in range(6):
            c0 = c_base + j * 512
            DMA("pool", [], [b_w[j]], semkey=f"dma_adaw{j}", out=wsl[j],
                in_=Dm["w_ada"][l, :, c0:c0 + 512].rearrange("(kt p) c -> p kt c", p=128))
        for j in range(6):
            w = wsl[j]
            pb = 6 + (j % 2)
            for kt in range(8):
                PE("matmul", [b_condB, b_w[j]], [bP[pb]], last=(kt == 7), out=pbank[pb][:], lhsT=condB[:, kt, :],
                   rhs=w[:, kt, :], start=(kt == 0), stop=(kt == 7))
            dst = ADA[:, j // 2, (j % 2) * 512:(j % 2) * 512 + 512]
            DVE("tensor_tensor", [bP[pb], bADA], [bADA], out=dst, in0=dst, in1=pbank[pb][:], op=ALU.add)
        DVE("scalar_tensor_tensor", [bADA, b_g], [bADA], out=ADA[:, 1, :], in0=ADA[:, 1, :], scalar=1.0, in1=gtmp,
            op0=ALU.add, op1=ALU.mult)

    stats_state = {"early": False}

    def stats_all(junk, early=False):
        if not early and stats_state["early"]:
            stats_state["early"] = False
            return
        if early:
            stats_state["early"] = True
        POOL("memset", [bSS], [bSS], ap=ss[:], constant=0.0)
        for tt in range(NT):
            ACT("activation", [bX[tt], bSS], [bSS], out=junk, in_=X[:, tt, :], func=AF.Square,
                accum_out=ss[:, tt:tt + 1])
        DVE("tensor_scalar", [bSS], [bSS], out=rstd[:], in0=ss[:], scalar1=1.0 / D, scalar2=EPS,
            op0=ALU.mult, op1=ALU.add)
        ACT("activation", [bSS], [bSS], out=rstd[:], in_=rstd[:], func=AF.Sqrt)
        DVE("reciprocal", [bSS], [bSS], out=rstd[:], in_=rstd[:])

    def norm_tile(tt, hbuf, b_h, junk, b_junk, mul_ap, mul_bufs):
        DVE("scalar_tensor_tensor", [bX[tt], bSS] + mul_bufs, [b_h], out=hbuf, in0=X[:, tt, :],
            scalar=rstd[:, tt:tt + 1], in1=mul_ap, op0=ALU.mult, op1=ALU.mult)

    def transpose_tile(h_bf, b_h, hT, b_hT, c0, pb):
        pv = pbank[pb][:].bitcast(BF16).rearrange("p (a b) -> p a b", a=8)
        for kt in range(8):
            PE("transpose", [b_h, bCONST], [bP[pb]], last=(kt == 7), out=pv[:, kt, :],
               in_=h_bf[:, kt * 128:(kt + 1) * 128], identity=ident_bf[:])
        evac(hT[:, :, c0:c0 + 128], pv, [bP[pb]], [b_hT])

    def zero_ss():
        pass

    def modulated_h_tile(tt, hbuf, b_h, hbf, b_hbf, junk, b_junk):
        norm_tile(tt, hbuf, b_h, junk, b_junk, ADA[:, 1, :], [bADA])
        POOL("tensor_tensor", [b_h, bADA], [b_hbf], out=hbf, in0=hbuf, in1=ADA[:, 0, :], op=ALU.add)

    def range_reduce(dst, src, tmpf, tmpi, bS):
        DVE("tensor_scalar", [bS], [bS], out=tmpf, in0=src, scalar1=1.0 / TWO_PI, scalar2=None, op0=ALU.mult)
        DVE("tensor_copy", [bS], [bS], out=tmpi, in_=tmpf)
        DVE("tensor_copy", [bS], [bS], out=tmpf, in_=tmpi)
        DVE("scalar_tensor_tensor", [bS], [bS], out=dst, in0=tmpf, scalar=-TWO_PI, in1=src, op0=ALU.mult, op1=ALU.add)
        DVE("tensor_scalar", [bS], [bS], out=dst, in0=dst, scalar1=3.1415925, scalar2=-3.1415925, op0=ALU.min,
            op1=ALU.max)

    def setup_ssm(l):
        B = {k: Buf("su_" + k) for k in (
            "lr", "li", "Br", "Bi", "Cs", "dt", "dsk", "xa", "tau", "angT", "xT", "E", "S", "C", "rr", "P", "k", "kt",
            "bb", "t16", "X12", "A12", "CT", "G", "tG", "Wbu", "X1r", "Wall", "KT8", "Wi", "cs8", "tab", "tabt", "cf")}
        G = aview(0, [128, 32, 9, 16], F32)
        WbuT = aview(18432, [128, 32, 8, 16], F32)
        X1rep = aview(34816, [128, 32, 8, 16], F32)
        KT8 = aview(51200, [128, 32, 128], F32)
        Wi = aview(67584, [128, 32, 128], F32)
        Wall = aview(83968, [128, 32, 4, 128], BF16)
        so = [116736]

        def small(shape, dt=F32):
            n = 4
            for s_ in shape[1:]:
                n *= s_
            v = aview(so[0], shape, dt)
            so[0] += n
            return v
        lrT, liT, dtt, xre, ang = [small([128, 32]) for _ in range(5)]
        nre, den, kre, kim, t32a, t32b, C8, S8, cw2 = [small([128, 32]) for _ in range(9)]
        dsk = small([128, 32])
        NTAU = 17
        tauI = small([128, 17], I32)
        tauv = small([128, 17])
        xo = [0]

        def xsmall(shape, dt=F32):
            n = 4
            for s_ in shape[1:]:
                n *= s_
            v = xview(xo[0], shape, dt)
            xo[0] += n
            return v
        Br2, Bi2, CreT2, CimT2, bb_re, bb_im, X1, X2, t16 = [xsmall([128, 32, 16]) for _ in range(9)]
        Csrc_re, Csrc_im = [xsmall([128, 4, 2, 64]) for _ in range(2)]
        tmpG = xsmall([128, 32, 9, 16])
        NTAU = 17
        angT, xT, Et, St, Ct, tmpf = [xsmall([128, 32, NTAU]) for _ in range(6)]
        tmpi = xsmall([128, 32, NTAU], I32)
        P17r, P17i = [xsmall([128, 32, NTAU]) for _ in range(2)]
        A1, A2 = [xsmall([128, 32, 9]) for _ in range(2)]
        P_re, P_im = P17r[:, :, 0:9], P17i[:, :, 0:9]
        PrR, PiR = P17r[:, :, 9:17], P17i[:, :, 9:17]

        for half in range(2):
            ps_ = slice(64 * half, 64 * half + 64)
            DMA("sp", [], [B["lr"]], semkey="dma_su_lr", out=lrT[ps_, :], in_=Dm["lam_re"][l].rearrange("g n -> n g"),
                allow_slow_non_contiguous=True)
            DMA("sp", [], [B["li"]], semkey="dma_su_li", out=liT[ps_, :], in_=Dm["lam_im"][l].rearrange("g n -> n g"),
                allow_slow_non_contiguous=True)
            DMA("sp", [], [B["Br"]], semkey="dma_su_Br", out=Br2[ps_, :, :], in_=Dm["b_re"][l].rearrange("g n h -> n g h"))
            DMA("sp", [], [B["Bi"]], semkey="dma_su_Bi", out=Bi2[ps_, :, :], in_=Dm["b_im"][l].rearrange("g n h -> n g h"))
            DMA("sp", [], [B["Cs"]], semkey="dma_su_Cs", out=Csrc_re[:, :, half, :],
                in_=Dm["c_re"][l].rearrange("g h n -> (g h) n").rearrange("(gt p) n -> p gt n", p=128))
            DMA("sp", [], [B["Cs"]], semkey="dma_su_Cs", out=Csrc_im[:, :, half, :],
                in_=Dm["c_im"][l].rearrange("g h n -> (g h) n").rearrange("(gt p) n -> p gt n", p=128))
        DMA("sp", [], [B["dt"]], semkey="dma_su_dt", out=dtt, in_=Dm["log_dt"][l].partition_broadcast(128))
        for j in range(8):
            DMA("sp", [], [B["dsk"]], semkey="dma_su_dsk", out=dsk[16 * j:16 * j + 16, :],
                in_=Dm["d_skip"][l].rearrange("g h -> h g"), allow_slow_non_contiguous=True)
        POOL("iota", [], [B["tau"]], out=tauI, pattern=[[1, NTAU]], base=0, channel_multiplier=0)
        DVE("tensor_copy", [B["tau"]], [B["tau"]], out=tauv, in_=tauI)
        DVE("tensor_scalar", [B["tau"]], [B["tau"]], out=tauv, in0=tauv, scalar1=-8.0, scalar2=None, op0=ALU.add)
        DVE("tensor_scalar", [B["tau"]], [B["tau"]], out=t32b[:, 0:NTAU], in0=tauv, scalar1=-1.0, scalar2=None, op0=ALU.mult)
        DVE("tensor_tensor", [B["tau"]], [B["tau"]], out=tauv, in0=tauv, in1=t32b[:, 0:NTAU], op=ALU.max)
        DVE("tensor_scalar", [B["tau"]], [B["tau"]], out=tauv, in0=tauv, scalar1=-1.0, scalar2=8.0, op0=ALU.mult,
            op1=ALU.add)
        ACT("activation", [B["dt"]], [B["dt"]], out=dtt, in_=dtt, func=AF.Exp)
        DVE("tensor_tensor", [B["lr"], B["dt"]], [B["xa"]], out=xre, in0=lrT, in1=dtt, op=ALU.mult)
        DVE("tensor_tensor", [B["li"], B["dt"], B["xa"]], [B["xa"]], out=ang, in0=liT, in1=dtt, op=ALU.mult)
        s3n = [128, 32, NTAU]
        tb_ = tauv.unsqueeze(1).to_broadcast(s3n)
        DVE("tensor_tensor", [B["xa"], B["tau"]], [B["angT"]], out=angT, in0=ang.unsqueeze(2).to_broadcast(s3n), in1=tb_,
            op=ALU.mult)
        DVE("tensor_tensor", [B["xa"], B["tau"]], [B["xT"]], out=xT, in0=xre.unsqueeze(2).to_broadcast(s3n), in1=tb_,
            op=ALU.mult)
        ACT("activation", [B["xT"]], [B["E"]], out=Et, in_=xT, func=AF.Exp)
        bR = Buf("su_rr1")
        range_reduce(St, angT, tmpf, tmpi, bR)
        ACT("activation", [bR, B["angT"]], [B["S"]], out=St, in_=St, func=AF.Sin)
        DVE("tensor_scalar", [B["angT"], bR], [B["angT"]], out=angT, in0=angT, scalar1=0.5 * math.pi, scalar2=None,
            op0=ALU.add)
        range_reduce(Ct, angT, tmpf, tmpi, bR)
        ACT("activation", [bR, B["angT"]], [B["C"]], out=Ct, in_=Ct, func=AF.Sin)
        DVE("tensor_tensor", [B["E"], B["C"]], [B["P"]], out=P17r, in0=Et, in1=Ct, op=ALU.mult)
        DVE("tensor_tensor", [B["E"], B["S"], B["P"]], [B["P"]], out=P17i, in0=Et, in1=St, op=ALU.mult)
        DVE("tensor_copy", [B["E"], B["tau"]], [B["cs8"], B["tau"]], out=t32b, in_=Et[:, :, 8])
        DMA("sp", [B["cs8"]], [], semkey="dma_setup_o", out=RHOD[l], in_=t32b)
        rk, wk = [B["P"], B["lr"], B["li"], B["k"], B["kt"]], [B["k"], B["kt"]]
        DVE("tensor_scalar", rk, wk, out=nre, in0=P_re[:, :, 1], scalar1=-1.0, scalar2=None, op0=ALU.add)
        nim = P_im[:, :, 1]
        DVE("tensor_tensor", rk, wk, out=den, in0=lrT, in1=lrT, op=ALU.mult)
        DVE("tensor_tensor", rk, wk, out=t32a, in0=liT, in1=liT, op=ALU.mult)
        DVE("tensor_tensor", rk, wk, out=den, in0=den, in1=t32a, op=ALU.add)
        DVE("reciprocal", rk, wk, out=den, in_=den)
        DVE("tensor_tensor", rk, wk, out=kre, in0=nre, in1=lrT, op=ALU.mult)
        DVE("tensor_tensor", rk, wk, out=t32a, in0=nim, in1=liT, op=ALU.mult)
        DVE("tensor_tensor", rk, wk, out=kre, in0=kre, in1=t32a, op=ALU.add)
        DVE("tensor_tensor", rk, wk, out=kre, in0=kre, in1=den, op=ALU.mult)
        DVE("tensor_tensor", rk, wk, out=kim, in0=nim, in1=lrT, op=ALU.mult)
        DVE("tensor_tensor", rk, wk, out=t32a, in0=nre, in1=liT, op=ALU.mult)
        DVE("tensor_tensor", rk, wk, out=kim, in0=kim, in1=t32a, op=ALU.subtract)
        DVE("tensor_tensor", rk, wk, out=kim, in0=kim, in1=den, op=ALU.mult)
        kre_b = kre.unsqueeze(2).to_broadcast([128, 32, 16])
        kim_b = kim.unsqueeze(2).to_broadcast([128, 32, 16])
        rb_, wb_ = [B["k"], B["Br"], B["Bi"], B["bb"], B["t16"]], [B["bb"], B["t16"]]
        DVE("tensor_tensor", rb_, wb_, out=bb_re, in0=Br2, in1=kre_b, op=ALU.mult)
        DVE("tensor_tensor", rb_, wb_, out=t16, in0=Bi2, in1=kim_b, op=ALU.mult)
        DVE("tensor_tensor", rb_, wb_, out=bb_re, in0=bb_re, in1=t16, op=ALU.subtract)
        DVE("tensor_tensor", rb_, wb_, out=bb_im, in0=Bi2, in1=kre_b, op=ALU.mult)
        DVE("tensor_tensor", rb_, wb_, out=t16, in0=Br2, in1=kim_b, op=ALU.mult)
        DVE("tensor_tensor", rb_, wb_, out=bb_im, in0=bb_im, in1=t16, op=ALU.add)
        lo, hi = slice(0, 64), slice(64, 128)
        rx, wx = [B["bb"], B["X12"]], [B["X12"]]
        DVE("tensor_copy", rx, wx, out=X1[lo], in_=bb_re[lo])
        DVE("tensor_copy", rx, wx, out=X1[hi], in_=bb_im[hi])
        DVE("tensor_scalar", rx, wx, out=X2[lo], in0=bb_im[lo], scalar1=-1.0, scalar2=None, op0=ALU.mult)
        DVE("tensor_copy", rx, wx, out=X2[hi], in_=bb_re[hi])
        ra, wa = [B["P"], B["A12"]], [B["A12"]]
        POOL("tensor_copy", ra, wa, out=A1[lo], in_=P_re[lo])
        POOL("tensor_scalar", ra, wa, out=A1[hi], in0=P_im[hi], scalar1=-1.0, scalar2=None, op0=ALU.mult)
        POOL("tensor_scalar", ra, wa, out=A2[lo], in0=P_im[lo], scalar1=-1.0, scalar2=None, op0=ALU.mult)
        POOL("tensor_scalar", ra, wa, out=A2[hi], in0=P_re[hi], scalar1=-1.0, scalar2=None, op0=ALU.mult)
        for bi_, (src, dst) in enumerate(((Csrc_re, CreT2), (Csrc_im, CimT2))):
            for gt in range(4):
                PE("transpose", [B["Cs"], bCONST], [bP[bi_]], out=pbank[bi_][:, gt * 128:(gt + 1) * 128],
                   in_=src[:, gt, :, :].rearrange("p a b -> p (a b)"), identity=ident_f[:])
            ACT("copy", [bP[bi_], B["CT"]], [B["CT"]], out=dst.rearrange("p g h -> p (g h)"), in_=pbank[bi_][:])
        s4 = [128, 32, 9, 16]
        DVE("tensor_tensor", [B["CT"], B["A12"]], [B["G"]], out=G, in0=CreT2.unsqueeze(2).to_broadcast(s4),
            in1=A1.unsqueeze(3).to_broadcast(s4), op=ALU.mult)
        DVE("tensor_tensor", [B["CT"], B["A12"]], [B["tG"]], out=tmpG, in0=CimT2.unsqueeze(2).to_broadcast(s4),
            in1=A2.unsqueeze(3).to_broadcast(s4), op=ALU.mult)
        DVE("tensor_tensor", [B["G"], B["tG"]], [B["G"]], out=G, in0=G, in1=tmpG, op=ALU.add)
        s4b = [128, 32, 8, 16]
        tmpW = tmpG[:, :, 0:8, :]
        DVE("tensor_tensor", [B["X12"], B["P"]], [B["Wbu"]], out=WbuT, in0=X1.unsqueeze(2).to_broadcast(s4b),
            in1=PrR.unsqueeze(3).to_broadcast(s4b), op=ALU.mult)
        DVE("tensor_tensor", [B["X12"], B["P"], B["G"], B["tG"]], [B["tG"]], out=tmpW, in0=X2.unsqueeze(2).to_broadcast(s4b),
            in1=PiR.unsqueeze(3).to_broadcast(s4b), op=ALU.mult)
        DVE("tensor_tensor", [B["Wbu"], B["tG"]], [B["Wbu"]], out=WbuT, in0=WbuT, in1=tmpW, op=ALU.add)
        POOL("tensor_copy", [B["X12"]], [B["X1r"]], out=X1rep, in_=X1.unsqueeze(2).to_broadcast(s4b))
        POOL("tensor_copy", [B["G"]], [B["Wall"]], out=Wall[:, :, 3, :].rearrange("p g (t h) -> p g t h", t=8),
             in_=G[:, :, 1:9, :])
        for g4 in range(8):
            b1, b2 = 2 + 2 * (g4 % 2), 3 + 2 * (g4 % 2)
            for gl in range(4):
                g = 4 * g4 + gl
                PE("transpose", [B["Wbu"], bCONST], [bP[b1]], last=False, out=pbank[b1][:, gl * 128:(gl + 1) * 128],
                   in_=WbuT[:, g, :, :].rearrange("p a b -> p (a b)"), identity=ident_f[:])
                PE("matmul", [B["X1r"], B["G"]], [bP[b2]], last=(gl == 3), out=pbank[b2][:, gl * 128:(gl + 1) * 128],
                   lhsT=X1rep[:, g, :, :].rearrange("p a b -> p (a b)"),
                   rhs=G[:, g, 0:8, :].rearrange("p a b -> p (a b)"), start=True, stop=True)
            ACT("copy", [bP[b1], B["Wall"]], [B["Wall"]], out=Wall[:, 4 * g4:4 * g4 + 4, 1, :],
                in_=pbank[b1][:].rearrange("p (g c) -> p g c", g=4))
            DVE("tensor_copy", [bP[b2], B["KT8"]], [B["KT8"]], out=KT8[:, 4 * g4:4 * g4 + 4, :],
                in_=pbank[b2][:].rearrange("p (g c) -> p g c", g=4))
        POOL("tensor_copy", [B["Wall"]], [B["Wall"]], out=Wall[:, :, 2, 0:64], in_=Wall[:, :, 1, 64:128])
        POOL("tensor_scalar", [B["Wall"]], [B["Wall"]], out=Wall[:, :, 2, 64:128], in0=Wall[:, :, 1, 0:64], scalar1=-1.0,
             scalar2=None, op0=ALU.mult)
        POOL("memset", [], [B["Wi"]], ap=Wi, constant=0.0)
        for j in range(8):
            DVE("scalar_tensor_tensor", [B["KT8"], B["Wi"], bCONST], [B["Wi"]], out=Wi[:, :, 16 * j:128],
                in0=KT8[:, :, 0:128 - 16 * j], scalar=jmask[:, j:j + 1], in1=Wi[:, :, 16 * j:128], op0=ALU.mult, op1=ALU.add)
        s3 = [128, 32, 128]
        DVE("tensor_tensor", [B["KT8"], B["Wi"], B["dsk"], bCONST], [B["KT8"]], out=KT8,
            in0=ident_f[:].unsqueeze(1).to_broadcast(s3), in1=dsk.unsqueeze(2).to_broadcast(s3), op=ALU.mult)
        DVE("tensor_tensor", [B["Wi"], B["KT8"], B["Wall"]], [B["Wall"]], out=Wall[:, :, 0, :], in0=Wi, in1=KT8, op=ALU.add)
        DMA("sp", [B["Wall"]], [], semkey="dma_setup_o", out=WSSM[l], in_=Wall)
        if debug and stop == "S":
            allb = list(B.values())
            dbg("Wall", Wall, [128, 32, 4, 128], BF16, allb)
            dbg("G", G, [128, 32, 9, 16], F32, allb)
            dbg("P_re", P_re, [128, 32, 9], F32, allb)
            dbg("P_im", P_im, [128, 32, 9], F32, allb)
            dbg("X1", X1, [128, 32, 16], F32, allb)
        DVE("tensor_scalar", [B["xa"], B["k"], B["kt"]], [B["kt"]], out=t32a, in0=ang, scalar1=8.0, scalar2=None, op0=ALU.mult)
        bR2 = Buf("su_rr2")
        DVE("tensor_copy", [B["kt"]], [bR2], out=cw2, in_=t32a)
        range_reduce(C8, cw2, S8, nre.bitcast(I32), bR2)
        S.barrier()
        bT = Buf("su_tab")
        rS, wS = [bT], [bT]
        Tc = aview(0, [128, 32, 256], F32)
        Ts = aview(32768, [128, 32, 256], F32)
        tbf = aview(65536, [128, 32, 256], F32)
        tbi = xview(0, [128, 32, 256], I32)
        cI = xview(32768, [128, 256], I32)
        cF = xview(32768 + 1024, [128, 256], F32)
        POOL("iota", rS, wS, out=cI, pattern=[[1, 256]], base=0, channel_multiplier=0)
        DVE("tensor_copy", rS, wS, out=cF, in_=cI)
        s3t = [128, 32, 256]
        DVE("tensor_tensor", rS, wS, out=Tc, in0=C8.unsqueeze(2).to_broadcast(s3t), in1=cF.unsqueeze(1).to_broadcast(s3t),
            op=ALU.mult)
        range_reduce(Ts, Tc, tbf, tbi, bT)
        ACT("activation", rS, wS, out=Ts, in_=Ts, func=AF.Sin)
        DMA("sp", rS, [], semkey="dma_setup_o", out=TAB[l, :, :, 1, :], in_=Ts)
        DVE("tensor_scalar", rS, wS, out=Tc, in0=Tc, scalar1=0.5 * math.pi, scalar2=None, op0=ALU.add)
        range_reduce(Tc, Tc, tbf, tbi, bT)
        ACT("activation", rS, wS, out=Tc, in_=Tc, func=AF.Sin)
        DMA("sp", rS, [], semkey="dma_setup_o", out=TAB[l, :, :, 0, :], in_=Tc)
        if debug and stop == "S":
            dbg("Tc", Tc, [128, 32, 256], F32, rS)
            dbg("Ts", Ts, [128, 32, 256], F32, rS)
        S.barrier()

    if do_setup:
        for l in range(n_layers):
            setup_ssm(l)


    b_conv = {nm: [Buf(f"cv_{nm}{e}") for e in range(NE)] for nm in ("w1", "w3", "w2")}
    conv_jobs = [(nm, e) for e in range(NE) for nm in ("w1", "w3", "w2")] if (sparse and n_layers > 1) else []
    conv_dst = {"w1": W1B, "w3": W3B, "w2": W2B}

    def issue_conv(n):
        for _ in range(min(n, len(conv_jobs))):
            nm, e = conv_jobs.pop(0)
            src = Dm[f"{nm}_moe"][0, e]
            dst = conv_dst[nm][e]
            rows = src.shape[0]
            hr = rows // 2
            for (a, b) in ((0, hr), (hr, rows)):
                DMA("pool", [], [b_conv[nm][e]], semkey=f"dma_cv_{nm}{e}", out=dst[a:b, :], in_=src[a:b, :])

    def sparse_moe(s, l):
        m = l // 2
        issue_conv(len(conv_jobs))
        h2T = aview(0, [128, 8, T], BF16)
        h2tok = aview(32768, [128, NT, D], BF16)
        hbuf2 = [aview(65536, [128, D], F32), aview(73728, [128, D], F32)]
        b_h2 = [Buf("h0"), Buf("h1")]
        junk = aview(69632, [128, D], BF16)
        sm = [118784]

        def small(shape, dt=F32):
            n = 2 if dt == BF16 else 4
            for s_ in shape[1:]:
                n *= s_
            n = (n + 3) // 4 * 4
            v = aview(sm[0], shape, dt)
            sm[0] += n
            return v
        s3 = [128, NT, 8]
        WR = small([128, 8, 8], BF16)
        logits, gates, sel, eq1, l2, Wn, TOT, PSa, PSb, val = [small(s3) for _ in range(10)]
        m1, m2, d1, d2, g1, g2 = [small([128, NT]) for _ in range(6)]
        d1i, d2i = small([128, NT], I32), small([128, NT], I32)
        brt, rowb, nev = small([128, 8]), small([128, 8]), small([128, 8])
        rowbi = small([128, 8], I32)
        thr = small([128, 4])
        thri = small([128, 4], I32)
        flagF = small([128, 8, 4])
        flagI = small([128, 8, 4], I32)
        selbf = small([128, 128], BF16)
        Ltri = small([128, 128], BF16)
        ones128 = small([128, 128], BF16)
        b_h2T, b_h, b_junk = Buf("h2T"), Buf("h"), Buf("junk")
        b_tok = [Buf(f"h2tok{i}") for i in range(NT)]
        b_WR, b_rt, b_cst, b_flag, b_HS, b_RS = Buf("WR"), Buf("router"), Buf("rcst"), Buf("flags"), Buf("HS"), Buf("RS")
        stats_all(junk)
        for tt in range(NT):
            modulated_h_tile(tt, hbuf2[tt % 2], b_h2[tt % 2], h2tok[:, tt, :], b_tok[tt], junk, b_junk)
            transpose_tile(h2tok[:, tt, :], b_tok[tt], h2T, b_h2T, tt * 128, 6 + (tt % 2))
        POOL("memset", [], [b_cst], ap=Ltri, constant=1.0)
        POOL("affine_select", [b_cst], [b_cst], out=Ltri, in_=Ltri, pattern=[[1, 128]], compare_op=ALU.is_gt, fill=0.0,
             base=0, channel_multiplier=-1)
        POOL("memset", [b_cst], [b_cst], ap=ones128, constant=1.0)
        POOL("iota", [b_cst], [b_cst], out=rowbi, pattern=[[T, 8]], base=0, channel_multiplier=0)
        POOL("iota", [b_cst], [b_cst], out=thri, pattern=[[512, 4]], base=0, channel_multiplier=0)
        DVE("tensor_copy", [b_cst], [b_cst], out=rowb, in_=rowbi)
        DVE("tensor_copy", [b_cst], [b_cst], out=thr, in_=thri)
        DMA("pool", [], [b_WR], semkey="dma_WR", out=WR, in_=Dm["w_router"][m].rearrange("(kt p) e -> p kt e", p=128))
        DMA("sp", [], [b_rt], semkey="dma_brt", out=brt, in_=Dm["b_router"][m].partition_broadcast(128))
        pv = pbank[0][:].rearrange("p (a b) -> p a b", a=NT)[:, :, 0:8]
        for tt in range(NT):
            for kt in range(8):
                PE("matmul", [b_WR, b_h2T], [bP[0]], last=(kt == 7 and tt == NT - 1), out=pv[:, tt, :],
                   lhsT=h2T[:, kt, tt * 128:(tt + 1) * 128], rhs=WR[:, kt, :], start=(kt == 0), stop=(kt == 7))
        rR, wR = [b_rt, b_cst], [b_rt]
        DVE("tensor_tensor", [bP[0], b_rt], wR, out=logits, in0=pv, in1=brt.unsqueeze(1).to_broadcast(s3), op=ALU.add)
        DVE("tensor_reduce", rR, wR, out=m1, in_=logits, axis=AX.X, op=ALU.max)
        DVE("tensor_tensor", rR, wR, out=eq1, in0=logits, in1=m1.unsqueeze(2).to_broadcast(s3), op=ALU.is_equal)
        DVE("scalar_tensor_tensor", rR, wR, out=l2, in0=eq1, scalar=-1.0e30, in1=logits, op0=ALU.mult, op1=ALU.add)
        DVE("tensor_reduce", rR, wR, out=m2, in_=l2, axis=AX.X, op=ALU.max)
        DVE("tensor_tensor", rR, wR, out=sel, in0=logits, in1=m2.unsqueeze(2).to_broadcast(s3), op=ALU.is_ge)
        DVE("tensor_tensor", rR, wR, out=l2, in0=logits, in1=m1.unsqueeze(2).to_broadcast(s3), op=ALU.subtract)
        ACT("activation", rR, wR, out=l2, in_=l2, func=AF.Exp)
        DVE("tensor_tensor", rR, wR, out=l2, in0=l2, in1=sel, op=ALU.mult)
        DVE("tensor_reduce", rR, wR, out=m2, in_=l2, axis=AX.X, op=ALU.add)
        DVE("reciprocal", rR, wR, out=m2, in_=m2)
        DVE("tensor_tensor", rR, wR, out=gates, in0=l2, in1=m2.unsqueeze(2).to_broadcast(s3), op=ALU.mult)
        DVE("tensor_copy", rR, wR, out=selbf, in_=sel.rearrange("p a b -> p (a b)"))
        PE("matmul", rR, [bP[1]], last=False, out=pbank[1][:, 0:128], lhsT=Ltri, rhs=selbf, start=True, stop=True)
        PE("matmul", rR, [bP[1]], last=True, out=pbank[1][:, 128:256], lhsT=ones128, rhs=selbf, start=True, stop=True)
        DVE("tensor_copy", [bP[1]] + rR, wR, out=Wn.rearrange("p a b -> p (a b)"), in_=pbank[1][:, 0:128])
        DVE("tensor_copy", [bP[1]] + rR, wR, out=TOT.rearrange("p a b -> p (a b)"), in_=pbank[1][:, 128:256])
        DVE("tensor_copy", rR, wR, out=PSa, in_=TOT)
        src_, dst_ = PSa, PSb
        for d_ in (1, 2, 4, 8):
            DVE("tensor_tensor", rR, wR, out=dst_[:, d_:, :], in0=src_[:, d_:, :], in1=src_[:, 0:NT - d_, :], op=ALU.add)
            DVE("tensor_copy", rR, wR, out=dst_[:, 0:d_, :], in_=src_[:, 0:d_, :])
            src_, dst_ = dst_, src_
        INC = src_
        DVE("tensor_copy", rR, wR, out=nev, in_=INC[:, NT - 1, :])
        DVE("tensor_tensor", rR, wR, out=val, in0=INC, in1=TOT, op=ALU.subtract)
        DVE("tensor_tensor", rR, wR, out=val, in0=val, in1=Wn, op=ALU.add)
        DVE("tensor_tensor", rR, wR, out=val, in0=val, in1=rowb.unsqueeze(1).to_broadcast(s3), op=ALU.add)
        DVE("tensor_tensor", rR, wR, out=l2, in0=val, in1=eq1, op=ALU.mult)
        DVE("tensor_reduce", rR, wR, out=d1, in_=l2, axis=AX.X, op=ALU.add)
        DVE("tensor_tensor", rR, wR, out=l2, in0=gates, in1=eq1, op=ALU.mult)
        DVE("tensor_reduce", rR, wR, out=g1, in_=l2, axis=AX.X, op=ALU.add)
        DVE("tensor_tensor", rR, wR, out=eq1, in0=sel, in1=eq1, op=ALU.subtract)
        DVE("tensor_tensor", rR, wR, out=l2, in0=val, in1=eq1, op=ALU.mult)
        DVE("tensor_reduce", rR, wR, out=d2, in_=l2, axis=AX.X, op=ALU.add)
        DVE("tensor_tensor", rR, wR, out=l2, in0=gates, in1=eq1, op=ALU.mult)
        DVE("tensor_reduce", rR, wR, out=g2, in_=l2, axis=AX.X, op=ALU.add)
        DVE("tensor_copy", rR, wR, out=d1i, in_=d1)
        DVE("tensor_copy", rR, wR, out=d2i, in_=d2)
        DVE("tensor_tensor", rR, [b_flag], out=flagF, in0=nev.unsqueeze(2).to_broadcast([128, 8, 4]),
            in1=thr.unsqueeze(1).to_broadcast([128, 8, 4]), op=ALU.is_gt)
        DVE("tensor_copy", [b_flag], [b_flag], out=flagI, in_=flagF)
        for tt in range(NT):
            for di in (d1i, d2i):
                b_HS = Buf("HS")
                S.dma("pool", dict(out=HS, out_offset=bass.IndirectOffsetOnAxis(ap=di[:, tt:tt + 1], axis=0),
                                   in_=h2tok[:, tt, :], in_offset=None),
                      [b_tok[tt], b_rt], [b_HS], semkey="dma_HS", method="indirect_dma_start")
        if debug and stop == "R":
            dbg("gates", gates, [128, NT, 8], F32, [b_rt])
            dbg("d1", d1, [128, NT], F32, [b_rt])
            dbg("d2", d2, [128, NT], F32, [b_rt])
            dbg("nev", nev, [128, 8], F32, [b_rt])
            dbg("flagI", flagI, [128, 8, 4], I32, [b_flag])
            return
        S.barrier(exclude=())
        actT = aview(0, [128, NFT, 512], BF16)
        W2 = aview(32768, [128, NFT, D], BF16)
        W1S = [aview(77824 + i * 4096, [128, 8, 256], BF16) for i in range(2)]
        W3S = [aview(77824 + 8192 + i * 4096, [128, 8, 256], BF16) for i in range(2)]
        h2Tc = aview(94208, [128, 8, 512], BF16)
        outs2 = [aview(102400 + i * 4096, [128, D], F32) for i in range(2)]
        HSc = aview(110592, [128, 4, D], BF16)
        sil = [aview(22528 + i * 2048, [128, 512], F32) for i in range(2)]
        b_act = [Buf(f"act{i}") for i in range(NFT)]
        b_W2 = [Buf(f"W2g{i}") for i in range(4)]
        b_W1S, b_W3S = [Buf(f"W1S{i}") for i in range(2)], [Buf(f"W3S{i}") for i in range(2)]
        b_outs2 = [Buf("outs0"), Buf("outs1")]
        b_HSc, b_h2Tc, b_sil = Buf("HSc"), Buf("h2Tc"), [Buf("sil0"), Buf("sil1")]
        st_it = 0
        slab = 0
        pe_it = 0
        ev_it = 0
        for e in range(NE):
            w1d, w3d, w2d = W1B[e], W3B[e], W2B[e]
            for c in range(4):
                r0 = e * T + c * 512
                S.cond_begin(flagI[0:1, e, c:c + 1], [b_flag])
                DMA("sp", [b_HS], [b_HSc], semkey="dma_HSc", out=HSc,
                    in_=HS[r0:r0 + 512, :].rearrange("(t p) d -> p t d", p=128))
                for gi, (f0, nf) in enumerate(FGROUPS):
                    DMA("pool", [b_conv["w2"][e]], [b_W2[gi]], semkey=f"dma_W2g{gi}", out=W2[:, f0:f0 + nf, :],
                        in_=w2d[f0 * 128:(f0 + nf) * 128, :].rearrange("(ft p) d -> p ft d", p=128))
                for t4 in range(4):
                    transpose_tile(HSc[:, t4, :], b_HSc, h2Tc, b_h2Tc, t4 * 128, 6 + (t4 % 2))
                for ft in range(NFT):
                    if ft % 2 == 0:
                        si = slab % 2
                        slab += 1
                        DMA("sp", [b_conv["w1"][e]], [b_W1S[si]], semkey=f"dma_W1S{si}", out=W1S[si],
                            in_=w1d[:, ft * 128:(ft + 2) * 128].rearrange("(kt p) c -> p kt c", p=128))
                        DMA("sp", [b_conv["w3"][e]], [b_W3S[si]], semkey=f"dma_W3S{si}", out=W3S[si],
                            in_=w3d[:, ft * 128:(ft + 2) * 128].rearrange("(kt p) c -> p kt c", p=128))
                    fsl = slice((ft % 2) * 128, (ft % 2) * 128 + 128)
                    pa, pb_ = (pe_it % 2) * 2, (pe_it % 2) * 2 + 1
                    sl_, b_sl = sil[pe_it % 2], b_sil[pe_it % 2]
                    pe_it += 1
                    for kt in range(8):
                        PE("matmul", [b_W1S[si], b_h2Tc], [bP[pa]], last=(kt == 7), out=pbank[pa][:],
                           lhsT=W1S[si][:, kt, fsl], rhs=h2Tc[:, kt, :], start=(kt == 0), stop=(kt == 7))
                    for kt in range(8):
                        PE("matmul", [b_W3S[si], b_h2Tc], [bP[pb_]], last=(kt == 7), out=pbank[pb_][:],
                           lhsT=W3S[si][:, kt, fsl], rhs=h2Tc[:, kt, :], start=(kt == 0), stop=(kt == 7))
                    ACT("activation", [bP[pa]], [b_sl], out=sl_, in_=pbank[pa][:], func=AF.Silu)
                    DVE("tensor_tensor", [b_sl, bP[pb_]], [b_act[ft]], out=actT[:, ft, :], in0=sl_, in1=pbank[pb_][:],
                        op=ALU.mult)
                for st_ in range(4):
                    ob2, b_ob2 = outs2[st_it % 2], b_outs2[st_it % 2]
                    for half in range(2):
                        bk = 4 + (ev_it % 4)
                        ev_it += 1
                        hsl = slice(half * 512, (half + 1) * 512)
                        for ft in range(NFT):
                            gi = 0 if ft < 6 else (1 if ft < 12 else (2 if ft < 17 else 3))
                            PE("matmul", [b_act[ft], b_W2[gi]], [bP[bk]], last=(ft == NFT - 1), out=pbank[bk][:],
                               lhsT=actT[:, ft, st_ * 128:(st_ + 1) * 128], rhs=W2[:, ft, hsl], start=(ft == 0),
                               stop=(ft == NFT - 1))
                        evac(ob2[:, hsl], pbank[bk][:], [bP[bk]], [b_ob2])
                    DMA("pool", [b_ob2], [b_RS], semkey=f"dma_RS{st_it % 2}",
                        out=RS[r0 + st_ * 128:r0 + (st_ + 1) * 128, :], in_=ob2)
                    st_it += 1
                S.cond_end()
        S.barrier()
        r1 = [aview(i * 4096, [128, D], F32) for i in range(2)]
        r2 = [aview(8192 + i * 4096, [128, D], F32) for i in range(2)]
        b_r1, b_r2 = [Buf("r10"), Buf("r11")], [Buf("r20"), Buf("r21")]
        for tt in range(NT):
            i2 = tt % 2
            S.dma("pool", dict(out=r1[i2], out_offset=None, in_=RS,
                               in_offset=bass.IndirectOffsetOnAxis(ap=d1i[:, tt:tt + 1], axis=0)),
                  [b_RS, b_rt], [b_r1[i2]], semkey=f"dma_r1{i2}", method="indirect_dma_start")
            S.dma("pool", dict(out=r2[i2], out_offset=None, in_=RS,
                               in_offset=bass.IndirectOffsetOnAxis(ap=d2i[:, tt:tt + 1], axis=0)),
                  [b_RS, b_rt], [b_r2[i2]], semkey=f"dma_r2{i2}", method="indirect_dma_start")
            DVE("tensor_scalar", [b_r1[i2], b_rt], [b_r1[i2]], out=r1[i2], in0=r1[i2], scalar1=g1[:, tt:tt + 1],
                scalar2=None, op0=ALU.mult)
            DVE("scalar_tensor_tensor", [b_r1[i2], b_r2[i2], b_rt], [b_r1[i2]], out=r1[i2], in0=r2[i2],
                scalar=g2[:, tt:tt + 1], in1=r1[i2], op0=ALU.mult, op1=ALU.add)
            DVE("tensor_tensor", [b_r1[i2], bADA], [b_r1[i2]], out=r1[i2], in0=r1[i2], in1=ADA[:, 2, :], op=ALU.mult)
            DVE("tensor_tensor", [b_r1[i2], bX[tt]], [bX[tt]], out=X[:, tt, :], in0=X[:, tt, :], in1=r1[i2], op=ALU.add)

    O_WIN = 0
    O_ATT = 0
    O_GT = 16384
    O_U8 = 32896
    O_QT = O_U8 + 16384
    O_KT = O_QT + 16384
    O_V = O_KT + 16384
    O_CS = O_V + 16384
    O_S6 = O_CS + 8192
    O_S7 = O_S6 + 8192

    done = (debug and stop == "S")
    for s in range(n_seq):
        if done:
            break
        load_x(s)
        for l in range(n_layers):
            WIN = aview(O_WIN, [128, 8, 2056], BF16)
            b_WIN = Buf("WIN")
            for g in range(4):
                DMA("pool", [], [b_WIN], semkey="dma_WIN", out=WIN[:, 2 * g:2 * g + 2, :],
                    in_=Dm["w_in"][l, 256 * g:256 * g + 256, 0:2056].rearrange("(kt p) c -> p kt c", p=128))
            stats_all(aview(O_CS, [128, D], BF16), early=True)
            compute_ada(s, l, 0, O_U8)
            issue_conv(3)
            S.barrier()
            U8 = aview(O_U8, [128, 2, 32, 8, 16], BF16)
            qT = aview(O_QT, [128, 4, T], BF16)
            kT = aview(O_KT, [128, 4, T], BF16)
            V = aview(O_V, [128, NT, 512], BF16)
            cs = aview(O_CS, [8, T], F32)
            hTc = aview(O_S6, [128, 8, 512], BF16)
            hbuf = aview(O_S7, [128, D], F32)
            hbf2 = [aview(O_S7 + 4096, [128, D], BF16), aview(O_S7 + 6144, [128, D], BF16)]
            junk = aview(O_S7 + 8192, [128, D], BF16)
            etmp = aview(O_S7 + 10240, [8, 512], F32)
            ltmp = aview(O_S7 + 12288, [8, 512], F32)
            negb = aview(O_S7 + 14336, [8, 1], F32)
            b_hbf2 = [Buf("hbf0"), Buf("hbf1")]
            stats_all(junk)
            b_U8, b_cs, b_hTc = [Buf("U8a"), Buf("U8b")], Buf("cs"), Buf("hTc")
            b_qT = [Buf(f"qT{i}") for i in range(4)]
            b_kT = [Buf(f"kT{i}") for i in range(4)]
            b_V = [Buf(f"V{i}") for i in range(NT)]
            b_h, b_hbf, b_junk, b_e, b_l, b_negb = Buf("h"), Buf("hbf"), Buf("junk"), Buf("e"), Buf("l"), Buf("negb")
            DMA("sp", [], [b_negb], semkey="dma_negb", out=negb, in_=Dm["b_forget"][l].rearrange("(h o) -> h o", o=1),
                allow_slow_non_contiguous=True)
            DVE("tensor_scalar", [b_negb], [b_negb], out=negb, in0=negb, scalar1=-1.0, scalar2=None, op0=ALU.mult)
            for c in range(4):
                for t4 in range(4):
                    tt = 4 * c + t4
                    modulated_h_tile(tt, hbuf, b_h, hbf2[tt % 2], b_hbf2[tt % 2], junk, b_junk)
                    transpose_tile(hbf2[tt % 2], b_hbf2[tt % 2], hTc, b_hTc, t4 * 128, 4 + (t4 % 2))
                cols = slice(c * 512, (c + 1) * 512)
                for which, dstT, b_dst, cbase in ((0, qT, b_qT, 0), (1, kT, b_kT, 512)):
                    for ft in range(4):
                        pb = ft
                        for kt in range(8):
                            PE("matmul", [b_WIN, b_hTc], [bP[pb]], last=(kt == 7), out=pbank[pb][:],
                               lhsT=WIN[:, kt, cbase + ft * 128:cbase + ft * 128 + 128], rhs=hTc[:, kt, :],
                               start=(kt == 0), stop=(kt == 7))
                        evac(dstT[:, ft, cols], pbank[pb][:], [bP[pb]], [b_dst[ft]])
                for t4 in range(4):
                    pb = t4
                    for kt in range(8):
                        PE("matmul", [b_WIN, b_hTc], [bP[pb]], last=(kt == 7), out=pbank[pb][:],
                           lhsT=hTc[:, kt, t4 * 128:(t4 + 1) * 128], rhs=WIN[:, kt, 1024:1536],
                           start=(kt == 0), stop=(kt == 7))
                    evac(V[:, 4 * c + t4, :], pbank[pb][:], [bP[pb]], [b_V[4 * c + t4]])
                for kt in range(8):
                    PE("matmul", [b_WIN, b_hTc], [bP[0]], last=(kt == 7), out=pbank[0][0:8, :],
                       lhsT=WIN[:, kt, 1536:1544], rhs=hTc[:, kt, :], start=(kt == 0), stop=(kt == 7))
                ACT("activation", [bP[0], b_negb], [b_e], out=etmp, in_=pbank[0][0:8, :], func=AF.Exp,
                    bias=negb[:, 0:1], scale=-1.0)
                ACT("activation", [b_e], [b_l], out=ltmp, in_=etmp, func=AF.Ln, bias=1.0, scale=1.0)
                init = 0.0 if c == 0 else cs[:, c * 512 - 1:c * 512]
                DVE("tensor_tensor_scan", [b_l, b_cs, bCONST], [b_cs], out=cs[:, c * 512:(c + 1) * 512],
                    data0=ones_f[0:8, 0:1].to_broadcast([8, 512]), data1=ltmp, initial=init, op0=ALU.mult, op1=ALU.add)
                half, po = c // 2, (c % 2) * 64
                for j in range(8):
                    pb = 1 + (j % 3)
                    for kt in range(8):
                        PE("matmul", [b_WIN, b_hTc], [bP[pb]], last=(kt == 7), out=pbank[pb][po:po + 64, :],
                           lhsT=hTc[:, kt, j::8], rhs=WIN[:, kt, 1544:2056], start=(kt == 0), stop=(kt == 7),
                           tile_position=(0, po))
                    evac(U8[po:po + 64, half, :, j, :], pbank[pb][po:po + 64, :].rearrange("p (g h) -> p g h", g=32),
                         [bP[pb]], [b_U8[half]])
            if debug and stop == "A":
                dbg("qT", qT, [128, 4, T], BF16, b_qT)
                dbg("kT", kT, [128, 4, T], BF16, b_kT)
                dbg("V", V, [128, NT, 512], BF16, b_V)
                dbg("U8", U8, [128, 2, 32, 8, 16], BF16, b_U8)
                dbg("cs", cs, [8, T], F32, [b_cs])
                dbg("ADA", ADA[:], [128, 3, D], F32, [bADA])
                done = True
                break
            S.barrier()
            issue_conv(4)
            attT = aview(O_ATT, [128, 4, T], BF16)
            b_attT = [Buf(f"attT{i}") for i in range(4)]
            Fs = aview(O_S6, [128, 3, T], BF16)
            O_B7 = O_S6 + 12288
            PT = [[aview(O_B7 + (hh * 3 + bf) * 1024, [128, 512], BF16) for bf in range(3)] for hh in range(2)]
            recip = [aview(O_B7 + 6144 + i * 2048, [128, 512], F32) for i in range(2)]
            cs_tok = aview(O_B7 + 10240, [128, NT, 8], F32)
            b_PT = [[Buf(f"PT{hh}{bf}") for bf in range(3)] for hh in range(2)]
            b_recip = [Buf("recip0"), Buf("recip1")]
            b_cstok, b_Fs = Buf("cs_tok"), Buf("Fs")
            hi = aview(O_GT, [8, T], BF16)
            mid = aview(O_GT + 4096, [8, T], BF16)
            lo_ = aview(O_GT + 8192, [8, T], BF16)
            r1 = aview(O_GT + 12288, [8, 512], F32)
            r2 = aview(O_GT + 14336, [8, 512], F32)
            b_split, b_r = Buf("split"), Buf("r12")
            if not (debug and stop in ("C", "Conly")):
                POOL("memset", [], [b_Fs], ap=Fs, constant=0.0)
                for c in range(4):
                    cl = slice(c * 512, (c + 1) * 512)
                    DVE("tensor_copy", [b_cs], [b_split], out=hi[:, cl], in_=cs[:, cl])
                    DVE("tensor_tensor", [b_cs, b_split], [b_r], out=r1, in0=cs[:, cl], in1=hi[:, cl], op=ALU.subtract)
                    DVE("tensor_copy", [b_r], [b_split], out=mid[:, cl], in_=r1)
                    DVE("tensor_tensor", [b_r, b_split], [b_r], out=r2, in0=r1, in1=mid[:, cl], op=ALU.subtract)
                    DVE("tensor_copy", [b_r], [b_split], out=lo_[:, cl], in_=r2)
                for h in range(H):
                    for i, piece in enumerate((hi, mid, lo_)):
                        p0 = 32 * (h % 3) + i
                        DMA("sp", [b_split], [b_Fs], semkey="dma_Fs", out=Fs[p0:p0 + 1, h // 3, :], in_=piece[h:h + 1, :])
                pv = pbank[0][:].rearrange("p (a b) -> p a b", a=NT)[:, :, 0:8]
                for tt in range(NT):
                    PE("transpose", [b_cs, bCONST], [bP[0]], last=(tt == NT - 1), out=pv[:, tt, :],
                       in_=cs[:, tt * 128:(tt + 1) * 128], identity=ident_f[0:8, 0:8])
                evac(cs_tok, pv, [bP[0]], [b_cstok], eng="dve")
                steps = []
                grp = 0
                for hp in range(4):
                    for Q in range(4):
                        for kb in range(4 * Q + 4):
                            steps.append((hp, Q, kb, grp, 4 * Q + 4))
                        grp += 1

                def stage1(i):
                    hp, Q, kb, g_, nkb = steps[i]
                    r = kb - 4 * Q
                    c0 = max(0, r) * 128
                    for hh in range(2):
                        sp_i = 3 * hh + (i % 3)
                        hs = slice(64 * hh, 64 * hh + 64)
                        PE("matmul", [b_kT[hp], b_qT[hp]], [bP[sp_i]], last=False, out=pbank[sp_i][:, c0:512],
                           lhsT=kT[hs, hp, kb * 128:(kb + 1) * 128], rhs=qT[hs, hp, Q * 512 + c0:(Q + 1) * 512],
                           start=True, stop=False)
                    for hh in range(2):
                        h = 2 * hp + hh
                        sp_i = 3 * hh + (i % 3)
                        fp0 = 32 * (h % 3)
                        PE("matmul", [b_Fs, bCONST], [bP[sp_i]], last=(r < 0 and hh == 1), out=pbank[sp_i][:, c0:512],
                           lhsT=neg8[fp0:fp0 + 3, :], rhs=Fs[fp0:fp0 + 3, h // 3, Q * 512 + c0:(Q + 1) * 512],
                           start=False, stop=True)
                    if r >= 0:
                        for hh in range(2):
                            sp_i = 3 * hh + (i % 3)
                            PE("matmul", [bCONST], [bP[sp_i]], last=(hh == 1), out=pbank[sp_i][:, c0:c0 + 128],
                               lhsT=ident_bf[:], rhs=maskneg[:], start=False, stop=True)
                    for hh in range(2):
                        h = 2 * hp + hh
                        sp_i = 3 * hh + (i % 3)
                        ACT("activation", [bP[sp_i], b_cstok], [b_PT[hh][i % 3]], out=PT[hh][i % 3][:, c0:512],
                            in_=pbank[sp_i][:, c0:512], func=AF.Exp, bias=cs_tok[:, kb, h:h + 1], scale=0.125)

                def stage2(i):
                    hp, Q, kb, g_, nkb = steps[i]
                    r = kb - 4 * Q
                    c0 = max(0, r) * 128
                    ob = 6
                    for hh in range(2):
                        h = 2 * hp + hh
                        hs = slice(64 * hh, 64 * hh + 64)
                        PE("matmul", [b_V[kb], b_PT[hh][i % 3]], [bP[ob]], last=False, out=pbank[ob][hs, c0:512],
                           lhsT=V[:, kb, h * 64:(h + 1) * 64], rhs=PT[hh][i % 3][:, c0:512], start=(kb == 0),
                           stop=(kb == nkb - 1), tile_position=(0, 64 * hh))
                    for hh in range(2):
                        hs = slice(64 * hh, 64 * hh + 64)
                        PE("matmul", [bCONST, b_PT[hh][i % 3]], [bP[ob + 1]], last=(hh == 1), out=pbank[ob + 1][hs, c0:512],
                           lhsT=ones_bf[:, 0:64], rhs=PT[hh][i % 3][:, c0:512], start=(kb == 0), stop=(kb == nkb - 1),
                           tile_position=(0, 64 * hh))
                    if kb == nkb - 1:
                        rc, b_rc = recip[g_ % 2], b_recip[g_ % 2]
                        DVE("reciprocal", [bP[ob + 1]], [b_rc], out=rc, in_=pbank[ob + 1][:])
                        DVE("tensor_tensor", [bP[ob], b_rc], [b_attT[hp]], out=attT[:, hp, Q * 512:(Q + 1) * 512],
                            in0=pbank[ob][:], in1=rc, op=ALU.mult)

                for w_ in range(24):
                    PE("matmul", [b_kT[0], b_qT[0]], [bP[6]], last=(w_ == 23), out=pbank[6][:], lhsT=kT[:, 0, 0:128],
                       rhs=qT[:, 0, 0:512], start=True, stop=True)
                stage1(0)
                stage1(1)
                for i in range(len(steps)):
                    if i + 2 < len(steps):
                        stage1(i + 2)
                    stage2(i)
            if debug and stop == "B":
                dbg("attT", attT, [128, 4, T], BF16, b_attT)
                done = True
                break
            S.barrier()
            issue_conv(3)
            gT = aview(O_GT, [128, 4, T], BF16)
            b_gT = [Buf(f"gT{i}") for i in range(4)]
            oc = [O_QT]

            def calloc(shape, dt):
                n = 2 if dt == BF16 else 4
                for s_ in shape[1:]:
                    n *= s_
                v = aview(oc[0], shape, dt)
                oc[0] += n
                return v
            WB = [calloc([128, 4, 4, 128], BF16) for _ in range(2)]
            TB = [calloc([128, 4, 2, 256], F32) for _ in range(2)]
            XG = [calloc([128, 4, 256], BF16) for _ in range(2)]
            wA, wB_, wC, wD = [calloc([128, 4, 256], F32) for _ in range(4)]
            Sp = [calloc([128, 4, 256], BF16) for _ in range(2)]
            Y8 = [calloc([128, 2, 8, 128], BF16) for _ in range(2)]
            gtmp_ = [calloc([128, 512], F32) for _ in range(2)]
            wglu = calloc([128, 4, 512], BF16)
            sg = calloc([128, 4, 512], BF16)
            bglu = calloc([128, 4], F32)
            b_WB, b_TB, b_XG = [Buf("WB0"), Buf("WB1")], [Buf("TB0"), Buf("TB1")], [Buf("XG0"), Buf("XG1")]
            b_wA, b_wB, b_wC, b_wD = Buf("wA"), Buf("wB"), Buf("wC"), Buf("wD")
            b_Sp, b_Y8, b_gtmp = [Buf("Sp0"), Buf("Sp1")], [Buf("Y80"), Buf("Y81")], [Buf("gtmp0"), Buf("gtmp1")]
            b_wglu, b_sg, b_bglu, b_rho = Buf("wglu"), Buf("sg"), Buf("bglu"), Buf("rho")
            DMA("sp", [], [b_rho], semkey="dma_rho", out=rho[:], in_=RHOD[l])
            DMA("pool", [], [b_wglu], semkey="dma_wglu", out=wglu,
                in_=Dm["w_glu"][l].rearrange("(kt p) c -> p kt c", p=128))
            DMA("sp", [], [b_bglu], semkey="dma_bglu", out=bglu, in_=Dm["b_glu"][l].rearrange("(kt p) -> p kt", p=128),
                allow_slow_non_contiguous=True)
            for i in range(2):
                POOL("memset", [], [b_Sp[i]], ap=Sp[i], constant=0.0)
            for bt in range(8):
                bi = bt % 2
                g0 = 4 * bt
                DMA("sp", [], [b_WB[bi]], semkey=f"dma_WB{bi}", out=WB[bi], in_=WSSM[l, :, g0:g0 + 4, :, :])
                DMA("sp", [], [b_TB[bi]], semkey=f"dma_TB{bi}", out=TB[bi], in_=TAB[l, :, g0:g0 + 4, :, :])
                pvb = pbank[0][:].bitcast(BF16)
                for gl in range(4):
                    g = g0 + gl
                    for half in range(2):
                        slot = gl * 2 + half
                        PE("transpose", [b_U8[half], bCONST], [bP[0]], last=(slot == 7),
                           out=pvb[:, slot * 128:(slot + 1) * 128], in_=U8[:, half, g, :, :].rearrange("p j h -> p (j h)"),
                           identity=ident_bf[:])
                evac(XG[bi].rearrange("p g c -> p (g c)"), pvb, [bP[0]], [b_XG[bi]])
                for gl in range(4):
                    bk = 1 + gl // 2
                    cs_ = slice((gl % 2) * 256, (gl % 2) * 256 + 256)
                    PE("matmul", [b_WB[bi], b_XG[bi]], [bP[bk]], out=pbank[bk][:, cs_], lhsT=WB[bi][:, gl, 1, :],
                       rhs=XG[bi][:, gl, :], start=True, stop=True)
                    PE("matmul", [b_WB[bi], b_XG[bi]], [bP[bk + 2]], out=pbank[bk + 2][:, cs_], lhsT=WB[bi][:, gl, 2, :],
                       rhs=XG[bi][:, gl, :], start=True, stop=True)
                for hb in range(2):
                    gs = slice(2 * hb, 2 * hb + 2)
                    Tc_ = TB[bi][:, gs, 0, :]
                    Ts_ = TB[bi][:, gs, 1, :]
                    Sl = pbank[1 + hb][:].rearrange("p (g c) -> p g c", g=2)
                    Sw = pbank[3 + hb][:].rearrange("p (g c) -> p g c", g=2)
                    DVE("tensor_tensor", [b_TB[bi], bP[1 + hb]], [b_wA], out=wA[:, gs, :], in0=Sl, in1=Tc_, op=ALU.mult)
                    DVE("tensor_tensor", [b_TB[bi], bP[3 + hb]], [b_wB], out=wB_[:, gs, :], in0=Sw, in1=Ts_, op=ALU.mult)
                    DVE("tensor_tensor", [b_wA, b_wB], [b_wA], out=wA[:, gs, :], in0=wA[:, gs, :], in1=wB_[:, gs, :],
                        op=ALU.add)
                    DVE("tensor_tensor", [b_TB[bi], bP[3 + hb]], [b_wB], out=wB_[:, gs, :], in0=Sw, in1=Tc_, op=ALU.mult)
                    DVE("tensor_tensor", [b_TB[bi], bP[1 + hb]], [b_wC], out=wC[:, gs, :], in0=Sl, in1=Ts_, op=ALU.mult)
                    DVE("tensor_tensor", [b_wB, b_wC], [b_wB], out=wB_[:, gs, :], in0=wB_[:, gs, :], in1=wC[:, gs, :],
                        op=ALU.subtract)
                for gl in range(4):
                    g = g0 + gl
                    rb = rho[:, g:g + 1].to_broadcast([128, 256])
                    DVE("tensor_tensor_scan", [b_wA, b_rho], [b_wC], out=wC[:, gl, :], data0=rb, data1=wA[:, gl, :],
                        initial=0.0, op0=ALU.mult, op1=ALU.add)
                    DVE("tensor_tensor_scan", [b_wB, b_rho], [b_wD], out=wD[:, gl, :], data0=rb, data1=wB_[:, gl, :],
                        initial=0.0, op0=ALU.mult, op1=ALU.add)
                Tc4 = TB[bi][:, :, 0, :]
                Ts4 = TB[bi][:, :, 1, :]
                POOL("tensor_tensor", [b_wC, b_TB[bi]], [b_wA], out=wA, in0=wC, in1=Tc4, op=ALU.mult)
                POOL("tensor_tensor", [b_wD, b_TB[bi]], [b_wB], out=wB_, in0=wD, in1=Ts4, op=ALU.mult)
                DVE("tensor_tensor", [b_wA, b_wB], [b_Sp[bi]], out=Sp[bi][:, :, 1:256], in0=wA[:, :, 0:255],
                    in1=wB_[:, :, 0:255], op=ALU.subtract)
                y8 = Y8[(bt // 2) % 2]
                b_y8 = b_Y8[(bt // 2) % 2]
                c0 = (g0 % 8) * 16
                for half in range(2):
                    bk = 5 + half
                    hsl = slice(half * 128, half * 128 + 128)
                    for gl in range(4):
                        osl = slice(gl * 128, gl * 128 + 128)
                        PE("matmul", [b_XG[bi], b_WB[bi]], [bP[bk]], last=False, out=pbank[bk][:, osl],
                           lhsT=XG[bi][:, gl, hsl], rhs=WB[bi][:, gl, 0, :], start=True, stop=False)
                        PE("matmul", [b_Sp[bi], b_WB[bi]], [bP[bk]], last=(gl == 3), out=pbank[bk][:, osl],
                           lhsT=Sp[bi][:, gl, hsl], rhs=WB[bi][:, gl, 3, :], start=False, stop=True)
                    gt_ = gtmp_[half]
                    b_gt = b_gtmp[half]
                    ACT("activation", [bP[bk]], [b_gt], out=gt_, in_=pbank[bk][:], func=AF.Square)
                    DVE("tensor_scalar", [b_gt], [b_gt], out=gt_, in0=gt_, scalar1=GELU_C * 0.044715, scalar2=GELU_C,
                        op0=ALU.mult, op1=ALU.add)
                    DVE("tensor_tensor", [b_gt, bP[bk]], [b_gt], out=gt_, in0=gt_, in1=pbank[bk][:], op=ALU.mult)
                    ACT("activation", [b_gt], [b_gt], out=gt_, in_=gt_, func=AF.Sigmoid)
                    DVE("tensor_tensor", [b_gt, bP[bk]], [b_y8],
                        out=y8[:, half, :, c0:c0 + 64].rearrange("p j (g h) -> p g j h", g=4),
                        in0=gt_.rearrange("p (g j h) -> p g j h", g=4, j=8),
                        in1=pbank[bk][:].rearrange("p (g j h) -> p g j h", g=4, j=8), op=ALU.mult)
                if bt % 2 == 1:
                    ct = bt // 2
                    for half in range(2):
                        bk = 7 if half == 0 else 0
                        pvt = pbank[bk][:].bitcast(BF16)
                        for j in range(8):
                            PE("transpose", [b_y8, bCONST], [bP[bk]], last=(j == 7), out=pvt[:, j * 128:(j + 1) * 128],
                               in_=y8[:, half, j, :], identity=ident_bf[:])
                        evac(gT[:, ct, half * 1024:(half + 1) * 1024].rearrange("p (c j) -> p j c", j=8),
                             pvt.rearrange("p (j c) -> p j c", j=8), [bP[bk]], [b_gT[ct]])
            if debug and stop == "C0":
                dbg("gT", gT, [128, 4, T], BF16, b_gT)
                done = True
                break
            for c in range(4):
                cl = slice(c * 512, (c + 1) * 512)
                for ot in range(4):
                    bk = 1 + ot
                    for ct in range(4):
                        PE("matmul", [b_wglu, b_gT[ct]], [bP[bk]], last=(ct == 3), out=pbank[bk][:],
                           lhsT=wglu[:, ct, ot * 128:(ot + 1) * 128], rhs=gT[:, ct, cl], start=(ct == 0), stop=(ct == 3))
                    ACT("activation", [bP[bk], b_bglu], [b_sg], out=sg[:, ot, :], in_=pbank[bk][:], func=AF.Sigmoid,
                        bias=bglu[:, ot:ot + 1], scale=1.0)
                for ot in range(4):
                    DVE("tensor_tensor", [b_sg, b_gT[ot]], [b_gT[ot]], out=gT[:, ot, cl], in0=gT[:, ot, cl],
                        in1=sg[:, ot, :], op=ALU.mult)
            if debug and stop == "C":
                dbg("ssT", gT, [128, 4, T], BF16, b_gT)
                done = True
                break
            S.barrier()
            zero_ss()
            hTc = aview(O_U8, [128, 8, 512], BF16)
            mixtmp = aview(O_U8 + 8192, [128, 8, 512], BF16)
            WG = aview(O_QT, [128, 8, 2048], BF16)
            WO = aview(O_QT, [128, 8, 1024], BF16)
            WPA = aview(O_V, [128, 4, 1024], BF16)
            WPS = aview(O_V + 8192, [128, 4, 1024], BF16)
            od = [O_CS]

            def dalloc(shape, dt):
                n = 2 if dt == BF16 else 4
                for s_ in shape[1:]:
                    n *= s_
                v = aview(od[0], shape, dt)
                od[0] += n
                return v
            hbuf = dalloc([128, D], F32)
            hbf2 = [dalloc([128, D], BF16) for _ in range(2)]
            b_hbf2 = [Buf("hbf0"), Buf("hbf1")]
            junk = dalloc([128, D], BF16)
            stats_all(junk)
            ga = [dalloc([128, 512], F32) for _ in range(2)]
            gsm = [dalloc([128, 512], F32) for _ in range(2)]
            t1_ = [dalloc([128, 512], F32) for _ in range(2)]
            t2_ = [dalloc([128, 512], F32) for _ in range(2)]
            xtmp = [dalloc([128, 512], F32) for _ in range(2)]
            bgate = dalloc([128, 16], F32)
            b_hTc, b_mix, b_WG, b_WPA, b_WPS = Buf("hTc"), Buf("mix"), Buf("WG"), Buf("WPA"), Buf("WPS")
            b_h, b_hbf, b_junk, b_bgate = Buf("h"), Buf("hbf"), Buf("junk"), Buf("bgate")
            b_ga, b_gsm = [Buf("ga0"), Buf("ga1")], [Buf("gs0"), Buf("gs1")]
            b_t1, b_t2, b_xtmp = [Buf("t10"), Buf("t11")], [Buf("t20"), Buf("t21")], [Buf("xt0"), Buf("xt1")]
            for g in range(4):
                DMA("pool", [], [b_WG], semkey="dma_WG", out=WG[:, 2 * g:2 * g + 2, :],
                    in_=Dm["w_in"][l, 256 * g:256 * g + 256, 2056:4104].rearrange("(kt p) c -> p kt c", p=128))
            DMA("pool", [], [b_WPA], semkey="dma_WPA", out=WPA,
                in_=Dm["w_proj_att"][l].rearrange("(kt p) c -> p kt c", p=128))
            DMA("pool", [], [b_WPS], semkey="dma_WPS", out=WPS,
                in_=Dm["w_proj_ssm"][l].rearrange("(kt p) c -> p kt c", p=128))
            DMA("sp", [], [b_bgate], semkey="dma_bgate", out=bgate, in_=Dm["b_gate"][l].rearrange("(ft p) -> p ft", p=128),
                allow_slow_non_contiguous=True)
            for c in range(4):
                cl = slice(c * 512, (c + 1) * 512)
                for t4 in range(4):
                    tt = 4 * c + t4
                    modulated_h_tile(tt, hbuf, b_h, hbf2[tt % 2], b_hbf2[tt % 2], junk, b_junk)
                    transpose_tile(hbf2[tt % 2], b_hbf2[tt % 2], hTc, b_hTc, t4 * 128, 6 + (t4 % 2))
                for dtile in range(8):
                    i2 = dtile % 2
                    for (gi, gbuf, b_g) in ((0, ga[i2], b_ga[i2]), (1, gsm[i2], b_gsm[i2])):
                        bk = gi + 4 * i2
                        col0 = gi * 1024 + dtile * 128
                        for kt in range(8):
                            PE("matmul", [b_WG, b_hTc], [bP[bk]], last=(kt == 7), out=pbank[bk][:],
                               lhsT=WG[:, kt, col0:col0 + 128], rhs=hTc[:, kt, :], start=(kt == 0), stop=(kt == 7))
                        ACT("activation", [bP[bk], b_bgate], [b_g], out=gbuf, in_=pbank[bk][:], func=AF.Sigmoid,
                            bias=bgate[:, gi * 8 + dtile:gi * 8 + dtile + 1], scale=1.0)
                    bka, bks = 2 + 4 * i2, 3 + 4 * i2
                    for ct in range(4):
                        PE("matmul", [b_WPA, b_attT[ct]], [bP[bka]], last=(ct == 3), out=pbank[bka][:],
                           lhsT=WPA[:, ct, dtile * 128:(dtile + 1) * 128], rhs=attT[:, ct, cl], start=(ct == 0),
                           stop=(ct == 3))
                    for ct in range(4):
                        PE("matmul", [b_WPS, b_gT[ct]], [bP[bks]], last=(ct == 3), out=pbank[bks][:],
                           lhsT=WPS[:, ct, dtile * 128:(dtile + 1) * 128], rhs=gT[:, ct, cl], start=(ct == 0),
                           stop=(ct == 3))
                    DVE("tensor_tensor", [b_ga[i2], bP[bka]], [b_t1[i2]], out=t1_[i2], in0=pbank[bka][:], in1=ga[i2],
                        op=ALU.mult)
                    DVE("tensor_tensor", [b_gsm[i2], bP[bks]], [b_t2[i2]], out=t2_[i2], in0=pbank[bks][:], in1=gsm[i2],
                        op=ALU.mult)
                    POOL("tensor_tensor", [b_t1[i2], b_t2[i2]], [b_mix], out=mixtmp[:, dtile, :], in0=t1_[i2],
                         in1=t2_[i2], op=ALU.add)
                ACT("copy", [b_mix], b_attT, out=attT[:, :, cl], in_=mixtmp[:, 0:4, :])
                POOL("tensor_copy", [b_mix], b_gT, out=gT[:, :, cl], in_=mixtmp[:, 4:8, :])
            for g in range(4):
                DMA("pool", [], [b_WG], semkey="dma_WG", out=WO[:, 2 * g:2 * g + 2, :],
                    in_=Dm["w_out"][l, 256 * g:256 * g + 256, :].rearrange("(kt p) c -> p kt c", p=128))
            it = 0
            for tt in range(NT):
                tsl = slice(tt * 128, (tt + 1) * 128)
                for half in range(2):
                    bk = it % 4
                    xt = xtmp[it % 2]
                    b_xt = b_xtmp[it % 2]
                    it += 1
                    for kt in range(8):
                        src = attT[:, kt, tsl] if kt < 4 else gT[:, kt - 4, tsl]
                        b_src = b_attT[kt] if kt < 4 else b_gT[kt - 4]
                        PE("matmul", [b_WG, b_src], [bP[bk]], last=(kt == 7), out=pbank[bk][:], lhsT=src,
                           rhs=WO[:, kt, half * 512:(half + 1) * 512], start=(kt == 0), stop=(kt == 7))
                    hsl = slice(half * 512, (half + 1) * 512)
                    DVE("tensor_tensor", [bP[bk], bADA], [b_xt], out=xt, in0=pbank[bk][:], in1=ADA[:, 2, hsl], op=ALU.mult)
                    POOL("tensor_tensor", [b_xt, bX[tt]], [bX[tt]], out=X[:, tt, hsl], in0=X[:, tt, hsl], in1=xt, op=ALU.add)
            if debug and stop == "D":
                dbg("X1", X[:], [128, NT, D], F32, bX)
                done = True
                break
            S.barrier()
            stats_all(aview(0, [128, D], BF16), early=True)
            compute_ada(s, l, 1, 32768)
            S.barrier()
            moe = (l % 2 == 1)
            if moe and sparse:
                sparse_moe(s, l)
                if debug and stop in ("R", f"E{l}"):
                    if stop != "R":
                        dbg("X2", X[:], [128, NT, D], F32, bX)
                    done = True
                    break
                S.barrier()
                continue
            h2T = aview(0, [128, 8, T], BF16)
            actT = aview(32768, [128, 6, T], BF16)
            oe = [57344]

            def ealloc(shape, dt):
                n = 2 if dt == BF16 else 4
                for s_ in shape[1:]:
                    n *= s_
                v = aview(oe[0], shape, dt)
                oe[0] += n
                return v
            W1S = [ealloc([128, 8, 128], BF16) for _ in range(3)]
            W3S = [ealloc([128, 8, 128], BF16) for _ in range(3)]
            W2G = [ealloc([128, 6, 1024], BF16) for _ in range(2)]
            hbuf = ealloc([128, D], F32)
            hbufb = ealloc([128, D], F32)
            b_hb = Buf("hb")
            hbf2 = [ealloc([128, D], BF16) for _ in range(2)]
            b_hbf2 = [Buf("hbf0"), Buf("hbf1")]
            junk = ealloc([128, D], BF16)
            stats_all(junk)
            sil = [ealloc([128, 512], F32) for _ in range(2)]
            xtmp = [ealloc([128, 512], F32) for _ in range(2)]
            WR = ealloc([128, 8, 8], BF16)
            logits = ealloc([128, NT, 8], F32)
            gates = ealloc([128, NT, 8], F32)
            eq = ealloc([128, NT, 8], F32)
            l2 = ealloc([128, NT, 8], F32)
            m1 = ealloc([128, NT], F32)
            m2 = ealloc([128, NT], F32)
            brt = ealloc([128, 8], F32)
            b_h2T, b_act = Buf("h2T"), [Buf(f"act{i}") for i in range(6)]
            b_W1S, b_W3S = [Buf(f"W1S{i}") for i in range(3)], [Buf(f"W3S{i}") for i in range(3)]
            b_W2G = [Buf("W2G0"), Buf("W2G1")]
            b_h, b_hbf, b_junk = Buf("h"), Buf("hbf"), Buf("junk")
            b_sil, b_xtmp = [Buf("sil0"), Buf("sil1")], [Buf("xt0"), Buf("xt1")]
            b_WR, b_rt = Buf("WR"), Buf("router")
            for tt in range(NT):
                modulated_h_tile(tt, (hbuf, hbufb)[tt % 2], (b_h, b_hb)[tt % 2], hbf2[tt % 2], b_hbf2[tt % 2], junk, b_junk)
                transpose_tile(hbf2[tt % 2], b_hbf2[tt % 2], h2T, b_h2T, tt * 128, 6 + (tt % 2))
            if moe:
                m = l // 2
                DMA("pool", [], [b_WR], semkey="dma_WR", out=WR, in_=Dm["w_router"][m].rearrange("(kt p) e -> p kt e", p=128))
                DMA("sp", [], [b_rt], semkey="dma_brt", out=brt, in_=Dm["b_router"][m].partition_broadcast(128))
                pv = pbank[0][:].rearrange("p (a b) -> p a b", a=NT)[:, :, 0:8]
                for tt in range(NT):
                    for kt in range(8):
                        PE("matmul", [b_WR, b_h2T], [bP[0]], last=(kt == 7 and tt == NT - 1), out=pv[:, tt, :],
                           lhsT=h2T[:, kt, tt * 128:(tt + 1) * 128], rhs=WR[:, kt, :], start=(kt == 0), stop=(kt == 7))
                s3 = [128, NT, 8]
                rR, wR = [b_rt], [b_rt]
                DVE("tensor_tensor", [bP[0], b_rt], wR, out=logits, in0=pv, in1=brt.unsqueeze(1).to_broadcast(s3), op=ALU.add)
                DVE("tensor_reduce", rR, wR, out=m1, in_=logits, axis=AX.X, op=ALU.max)
                DVE("tensor_tensor", rR, wR, out=eq, in0=logits, in1=m1.unsqueeze(2).to_broadcast(s3), op=ALU.is_equal)
                DVE("scalar_tensor_tensor", rR, wR, out=l2, in0=eq, scalar=-1.0e30, in1=logits, op0=ALU.mult, op1=ALU.add)
                DVE("tensor_reduce", rR, wR, out=m2, in_=l2, axis=AX.X, op=ALU.max)
                DVE("tensor_tensor", rR, wR, out=eq, in0=logits, in1=m2.unsqueeze(2).to_broadcast(s3), op=ALU.is_ge)
                DVE("tensor_tensor", rR, wR, out=l2, in0=logits, in1=m1.unsqueeze(2).to_broadcast(s3), op=ALU.subtract)
                ACT("activation", rR, wR, out=l2, in_=l2, func=AF.Exp)
                DVE("tensor_tensor", rR, wR, out=l2, in0=l2, in1=eq, op=ALU.mult)
                DVE("tensor_reduce", rR, wR, out=m2, in_=l2, axis=AX.X, op=ALU.add)
                DVE("reciprocal", rR, wR, out=m2, in_=m2)
                DVE("tensor_tensor", rR, wR, out=gates, in0=l2, in1=m2.unsqueeze(2).to_broadcast(s3), op=ALU.mult)
                if debug and stop == "R":
                    dbg("gates", gates, [128, NT, 8], F32, [b_rt])
                    done = True
                    break
            n_exp = NE if moe else 1
            slab = 0
            gcount = 0
            pe_it = 0
            ev_it = 0
            for e in range(n_exp):
                if moe:
                    w1d, w3d, w2d = Dm["w1_moe"][0, e], Dm["w3_moe"][0, e], Dm["w2_moe"][0, e]
                else:
                    w1d, w3d, w2d = Dm["w1_dense"][l // 2], Dm["w3_dense"][l // 2], Dm["w2_dense"][l // 2]
                for (f0, nf) in FGROUPS:
                    gb = gcount % 2
                    gcount += 1
                    DMA("pool", [], [b_W2G[gb]], semkey=f"dma_W2G{gb}", out=W2G[gb][:, 0:nf, :],
                        in_=w2d[f0 * 128:(f0 + nf) * 128, :].rearrange("(ft p) d -> p ft d", p=128))
                    for fl in range(nf):
                        ft = f0 + fl
                        si = slab % 3
                        slab += 1
                        DMA("pool", [], [b_W1S[si]], semkey=f"dma_W1S{si}", out=W1S[si],
                            in_=w1d[:, ft * 128:(ft + 1) * 128].rearrange("(kt p) c -> p kt c", p=128))
                        DMA("pool", [], [b_W3S[si]], semkey=f"dma_W3S{si}", out=W3S[si],
                            in_=w3d[:, ft * 128:(ft + 1) * 128].rearrange("(kt p) c -> p kt c", p=128))
                        for c in range(4):
                            cl = slice(c * 512, (c + 1) * 512)
                            pa, pb_ = (pe_it % 2) * 2, (pe_it % 2) * 2 + 1
                            sl_, b_sl = sil[pe_it % 2], b_sil[pe_it % 2]
                            pe_it += 1
                            for kt in range(8):
                                PE("matmul", [b_W1S[si], b_h2T], [bP[pa]], last=(kt == 7), out=pbank[pa][:],
                                   lhsT=W1S[si][:, kt, :], rhs=h2T[:, kt, cl], start=(kt == 0), stop=(kt == 7))
                            for kt in range(8):
                                PE("matmul", [b_W3S[si], b_h2T], [bP[pb_]], last=(kt == 7), out=pbank[pb_][:],
                                   lhsT=W3S[si][:, kt, :], rhs=h2T[:, kt, cl], start=(kt == 0), stop=(kt == 7))
                            ACT("activation", [bP[pa]], [b_sl], out=sl_, in_=pbank[pa][:], func=AF.Silu)
                            DVE("tensor_tensor", [b_sl, bP[pb_]], [b_act[fl]], out=actT[:, fl, cl], in0=sl_,
                                in1=pbank[pb_][:], op=ALU.mult)
                    for tt in range(NT):
                        tsl = slice(tt * 128, (tt + 1) * 128)
                        for half in range(2):
                            bk = 4 + (ev_it % 4)
                            xt, b_xt = xtmp[ev_it % 2], b_xtmp[ev_it % 2]
                            ev_it += 1
                            hsl = slice(half * 512, (half + 1) * 512)
                            for fl in range(nf):
                                PE("matmul", [b_act[fl], b_W2G[gb]], [bP[bk]], last=(fl == nf - 1), out=pbank[bk][:],
                                   lhsT=actT[:, fl, tsl], rhs=W2G[gb][:, fl, hsl], start=(fl == 0), stop=(fl == nf - 1))
                            if moe:
                                DVE("scalar_tensor_tensor", [bP[bk], b_rt, bADA], [b_xt], out=xt, in0=pbank[bk][:],
                                    scalar=gates[:, tt, e:e + 1], in1=ADA[:, 2, hsl], op0=ALU.mult, op1=ALU.mult)
                            else:
                                DVE("tensor_tensor", [bP[bk], bADA], [b_xt], out=xt, in0=pbank[bk][:], in1=ADA[:, 2, hsl],
                                    op=ALU.mult)
                            DVE("tensor_tensor", [b_xt, bX[tt]], [bX[tt]], out=X[:, tt, hsl], in0=X[:, tt, hsl], in1=xt,
                                op=ALU.add)
            if debug and stop == f"E{l}":
                dbg("X2", X[:], [128, NT, D], F32, bX)
                done = True
                break
            S.barrier()
        if done:
            break
        if n_layers == DEPTH:
            zero_ss()
            gfin = aview(0, [128, D], F32)
            junk = aview(4096, [128, D], BF16)
            ob_ = [aview(8192 + i * 4096, [128, D], F32) for i in range(2)]
            b_gf, b_junk, b_ob = Buf("gfin"), Buf("junk"), [Buf("ob0"), Buf("ob1")]
            DMA("sp", [], [b_gf], semkey="dma_gfin", out=gfin, in_=Dm["g_final"].partition_broadcast(128))
            stats_all(junk)
            for tt in range(NT):
                norm_tile(tt, ob_[tt % 2], b_ob[tt % 2], junk, b_junk, gfin, [b_gf])
                DMA("sp", [b_ob[tt % 2]], [], semkey=f"dma_out{tt % 2}", out=out_d[s, tt * 128:(tt + 1) * 128, :],
                    in_=ob_[tt % 2])
            S.barrier()

    S.barrier()
    S.run()
    st.close()
    return nc, dbg_out


_CACHE = {}


def kernel(**inputs):
    if "nc" not in _CACHE:
        _CACHE["nc"] = build()[0]
    nc = _CACHE["nc"]
    in_maps = []
    for i in range(NCORES):
        m = {}
        for k in IN_SPECS:
            a = np.asarray(inputs[k], dtype=np.float32)
            if k in ("x", "c"):
                a = a[2 * i:2 * i + 2]
            m[k] = np.ascontiguousarray(a)
        in_maps.append(m)
    res = run_bass_kernel_spmd(nc, in_maps, core_ids=list(range(NCORES)))
    return np.concatenate([np.asarray(r["out"]) for r in res.results], axis=0).astype(np.float32)
```

```python
import math
from contextlib import ExitStack
import numpy as np
import concourse.bass as bass
import concourse.mybir as mybir
from concourse.bass_utils import run_bass_kernel_spmd

F32 = mybir.dt.float32
BF16 = mybir.dt.bfloat16
I32 = mybir.dt.int32
AF = mybir.ActivationFunctionType
ALU = mybir.AluOpType
AX = mybir.AxisListType

NCORES = 8
D = 1024
T = 2048
NT = 16
DEPTH = 2
H = 8
DFF = 2816
NFT = 22
NE = 8
N_IN = 4104
EPS = 1e-6
NEGBIG = -30000.0
TWO_PI = 2.0 * math.pi
GELU_C = 1.5957691216057308
FGROUPS = [(0, 6), (6, 6), (12, 5), (17, 5)]

ENGINES = ("pe", "act", "dve", "pool", "sp")

IN_SPECS = {
    "x": [2, T, D], "c": [2, D], "w_ada": [2, D, 6 * D], "b_ada": [2, 6 * D], "g_mix": [2, D],
    "w_in": [2, D, N_IN], "b_forget": [2, 8], "b_gate": [2, 2 * D], "lam_re": [2, 32, 64],
    "lam_im": [2, 32, 64], "log_dt": [2, 32], "b_re": [2, 32, 64, 16], "b_im": [2, 32, 64, 16],
    "c_re": [2, 32, 16, 64], "c_im": [2, 32, 16, 64], "d_skip": [2, 32, 16], "w_glu": [2, 512, 512],
    "b_glu": [2, 512], "w_proj_att": [2, 512, D], "w_proj_ssm": [2, 512, D], "w_out": [2, D, D],
    "g_ffn": [2, D], "w1_dense": [1, D, DFF], "w3_dense": [1, D, DFF], "w2_dense": [1, DFF, D],
    "w_router": [1, D, 8], "b_router": [1, 8], "w1_moe": [1, 8, D, DFF], "w3_moe": [1, 8, D, DFF],
    "w2_moe": [1, 8, DFF, D], "g_final": [D],
}


class Buf:
    __slots__ = ("name", "w", "r")

    def __init__(self, name):
        self.name = name
        self.w = None
        self.r = {}


class Sched:
    def __init__(self, nc, stack):
        self.nc = nc
        self.stack = stack
        self.ops = {e: [] for e in ENGINES}
        self.cnt = {}
        self.known = {e: {} for e in ENGINES}
        self.sems = {}
        self.pe_pending = []
        self.cur = None

    def sem(self, key):
        if key not in self.sems:
            self.sems[key] = self.stack.enter_context(self.nc.semaphore(str(key)))
            self.cnt[key] = 0
        return self.sems[key]

    def _collect(self, eng, reads, writes):
        waits = {}
        for b in reads:
            if b.w is not None and waits.get(b.w[0], 0) < b.w[1]:
                waits[b.w[0]] = b.w[1]
        for b in writes:
            if b.w is not None and waits.get(b.w[0], 0) < b.w[1]:
                waits[b.w[0]] = b.w[1]
            for k, v in b.r.items():
                if waits.get(k, 0) < v:
                    waits[k] = v
        out = []
        kn = self.known[eng]
        for k, v in waits.items():
            if kn.get(k, 0) < v:
                kn[k] = v
                out.append((k, v))
        return out

    @staticmethod
    def _mark(ev, reads, writes):
        for b in writes:
            b.w = ev
            b.r = {}
        for b in reads:
            if b.r.get(ev[0], 0) < ev[1]:
                b.r[ev[0]] = ev[1]

    def op(self, eng, method, kw, reads=(), writes=()):
        waits = self._collect(eng, reads, writes)
        self.sem(eng)
        self.cnt[eng] += 1
        ev = (eng, self.cnt[eng])
        self._emit(eng, (waits, method, kw, eng, 1))
        self._mark(ev, reads, writes)
        return ev

    def _emit(self, eng, rec):
        if self.cur is None:
            self.ops[eng].append(rec)
        else:
            self.cur["ops"][eng].append(rec)
            if rec[3] is not None:
                d = self.cur["incs"][eng]
                d[rec[3]] = d.get(rec[3], 0) + rec[4]

    def cond_begin(self, flag_ap, reads):
        assert self.cur is None and not self.pe_pending
        snap = {e: dict(self.known[e]) for e in ENGINES}
        waits, after = {}, {}
        for e in ENGINES:
            waits[e] = self._collect(e, reads, ())
            after[e] = dict(self.known[e])
        self.cur = {"flag": flag_ap, "ops": {e: [] for e in ENGINES}, "incs": {e: {} for e in ENGINES},
                    "waits": waits, "snap": snap, "after": after, "base": dict(self.cnt)}

    def cond_end(self):
        assert not self.pe_pending
        blk = self.cur
        self.cur = None
        for e in ENGINES:
            if blk["ops"][e]:
                base = {k: blk["base"].get(k, 0) for k in blk["incs"][e]}
                self.ops[e].append(("COND", blk["flag"], blk["waits"][e], blk["ops"][e], blk["incs"][e], base))
                self.known[e] = blk["after"][e]
            else:
                self.known[e] = blk["snap"][e]

    def pe(self, method, kw, reads=(), writes=(), last=True):
        if not last:
            waits = self._collect("pe", reads, writes)
            self._emit("pe", (waits, method, kw, None, 0))
            self.pe_pending.append((list(reads), list(writes)))
            return None
        ev = self.op("pe", method, kw, reads, writes)
        for (r, w) in self.pe_pending:
            self._mark(ev, r, w)
        self.pe_pending = []
        return ev

    def dma(self, queue, kw, reads=(), writes=(), semkey=None, method="dma_start"):
        waits = self._collect(queue, reads, writes)
        if semkey is None:
            semkey = "dma_" + (writes[0].name if writes else reads[0].name)
        self.sem(semkey)
        self.cnt[semkey] += 16
        ev = (semkey, self.cnt[semkey])
        self._emit(queue, (waits, method, kw, semkey, 16))
        self._mark(ev, reads, writes)
        return ev

    def barrier(self, exclude=()):
        assert not self.pe_pending
        for e in ENGINES:
            waits = []
            kn = self.known[e]
            for k, v in self.cnt.items():
                if k in exclude:
                    continue
                if kn.get(k, 0) < v:
                    kn[k] = v
                    waits.append((k, v))
            if waits:
                self.ops[e].append((waits, None, None, None, 0))

    def run(self):
        nc = self.nc
        ops = self.ops
        sems = self.sems

        regs = {}

        def replay(e, lst):
            for rec in lst:
                if rec[0] == "COND":
                    _, flag_ap, waits, inner, incs, base = rec
                    for (k, v) in waits:
                        e.wait_ge(sems[k], v)
                    regs["n"] = regs.get("n", 0) + 1
                    reg = e.alloc_register(f"condreg{regs['n']}")
                    e.reg_load(reg, flag_ap)
                    cv = e.snap(reg, donate=True)
                    with e.If(cv > 0):
                        replay(e, inner)
                    with e.Else():
                        for k, n in incs.items():
                            if base[k] > 0:
                                e.wait_ge(sems[k], base[k])
                            e.sem_inc(sems[k], n)
                    try:
                        nc.free_register(reg)
                    except Exception:
                        pass
                    continue
                (waits, method, kw, key, n) = rec
                for (k, v) in waits:
                    e.wait_ge(sems[k], v)
                if method is None:
                    continue
                ins = getattr(e, method)(**kw)
                if key is not None:
                    ins.then_inc(sems[key], n)

        with nc.Block() as block:
            @block.tensor
            def _(e):
                replay(e, ops["pe"])

            @block.scalar
            def _(e):
                replay(e, ops["act"])

            @block.vector
            def _(e):
                replay(e, ops["dve"])

            @block.gpsimd
            def _(e):
                replay(e, ops["pool"])

            @block.sync
            def _(e):
                replay(e, ops["sp"])


def build(debug=False, stop=None, n_seq=2, n_layers=DEPTH, do_setup=True, sparse=True):
    nc = bass.Bass("TRN2", target_bir_lowering=False)
    Dm = {k: nc.dram_tensor(k, shp, F32, kind="ExternalInput").ap() for k, shp in IN_SPECS.items()}
    out_d = nc.dram_tensor("out", [2, T, D], F32, kind="ExternalOutput").ap()
    WSSM = nc.dram_tensor("scr_wssm", [2, 128, 32, 4, 128], BF16, kind="Internal").ap()
    TAB = nc.dram_tensor("scr_tab", [2, 128, 32, 2, 256], F32, kind="Internal").ap()
    RHOD = nc.dram_tensor("scr_rho", [2, 128, 32], F32, kind="Internal").ap()
    HS = nc.dram_tensor("scr_hs", [NE * T, D], BF16, kind="Internal").ap()
    W1B = nc.dram_tensor("scr_w1b", [NE, D, DFF], BF16, kind="Internal").ap()
    W3B = nc.dram_tensor("scr_w3b", [NE, D, DFF], BF16, kind="Internal").ap()
    W2B = nc.dram_tensor("scr_w2b", [NE, DFF, D], BF16, kind="Internal").ap()
    RS = nc.dram_tensor("scr_rs", [NE * T, D], F32, kind="Internal").ap()
    dbg_out = {}

    st = ExitStack()
    S = Sched(nc, st)
    sb = lambda name, shape, dt: st.enter_context(nc.sbuf_tensor(name, shape, dt))

    def DVE(method, r, w, **kw):
        return S.op("dve", method, kw, r, w)

    def ACT(method, r, w, **kw):
        return S.op("act", method, kw, r, w)

    def POOL(method, r, w, **kw):
        return S.op("pool", method, kw, r, w)

    def PE(method, r, w, last=True, **kw):
        return S.pe(method, kw, r, w, last=last)

    def DMA(queue, r, w, semkey=None, **kw):
        return S.dma(queue, kw, r, w, semkey=semkey)

    X = sb("X", [128, NT, D], F32)
    bX = [Buf(f"X{t}") for t in range(NT)]
    ADA = sb("ADA", [128, 3, D], F32)
    bADA = Buf("ADA")
    ident_bf = sb("ident_bf", [128, 128], BF16)
    ident_f = sb("ident_f", [128, 128], F32)
    ones_bf = sb("ones_bf", [128, 64], BF16)
    neg8 = sb("neg8", [128, 128], BF16)
    maskneg = sb("maskneg", [128, 128], BF16)
    ones_f = sb("ones_f", [128, 1], F32)
    jmask = sb("jmask", [128, 8], F32)
    ss = sb("ss", [128, NT], F32)
    rstd = sb("rstd", [128, NT], F32)
    rho = sb("rho", [128, 32], F32)
    bCONST = Buf("const")
    bSS = Buf("ss")
    ARENA_BYTES = 131072
    arena = sb("arena", [128, ARENA_BYTES // 4], F32)
    pbank = [st.enter_context(nc.psum_tensor(f"pb{i}", [128, 512], F32)) for i in range(8)]
    bP = [Buf(f"pb{i}") for i in range(8)]
    Xflat = X[:].rearrange("p a b -> p (a b)")

    def mkview(flat, cap, off, shape, dt):
        size = 2 if dt == BF16 else 4
        n = 1
        for s_ in shape[1:]:
            n *= s_
        assert off % 4 == 0 and (n * size) % 4 == 0 and off + n * size <= cap, (off, shape, cap)
        v = flat[0:shape[0], off // 4:(off + n * size) // 4]
        if dt != F32:
            v = v.bitcast(dt)
        if len(shape) > 2:
            names = [f"f{i}" for i in range(len(shape) - 1)]
            kw = {nm: s_ for nm, s_ in zip(names[:-1], shape[1:-1])}
            v = v.rearrange(f"p ({' '.join(names)}) -> p {' '.join(names)}", **kw)
        return v

    def aview(off, shape, dt):
        return mkview(arena, ARENA_BYTES, off, shape, dt)

    def xview(off, shape, dt):
        return mkview(Xflat, NT * D * 4, off, shape, dt)

    def dbg(name, ap, shape, dt, reads):
        if not debug:
            return
        t = nc.dram_tensor("dbg_" + name, shape, dt, kind="ExternalOutput").ap()
        dbg_out[name] = t
        DMA("sp", reads, [], semkey="dbg", out=t, in_=ap)

    def diag_select(t, op, base=0, cm=1, step=-1, n=128):
        POOL("affine_select", [bCONST], [bCONST], out=t, in_=t, pattern=[[step, n]], compare_op=op, fill=0.0,
             base=base, channel_multiplier=cm)

    POOL("memset", [], [bCONST], ap=ident_bf[:], constant=1.0)
    diag_select(ident_bf[:], ALU.is_equal)
    POOL("memset", [], [bCONST], ap=ident_f[:], constant=1.0)
    diag_select(ident_f[:], ALU.is_equal)
    POOL("memset", [], [bCONST], ap=ones_bf[:], constant=1.0)
    POOL("memset", [], [bCONST], ap=neg8[:], constant=-8.0)
    POOL("memset", [], [bCONST], ap=ones_f[:], constant=1.0)
    POOL("memset", [], [bCONST], ap=maskneg[:], constant=NEGBIG)
    diag_select(maskneg[:], ALU.is_gt)
    POOL("memset", [], [bCONST], ap=jmask[:], constant=1.0)
    diag_select(jmask[:], ALU.is_ge, base=0, cm=1, step=-16, n=8)
    diag_select(jmask[:], ALU.is_ge, base=15, cm=-1, step=16, n=8)

    evac_rr = [0]

    def evac(out_ap, in_ap, reads, writes, eng=None):
        if eng is None:
            eng = "act" if evac_rr[0] % 2 == 0 else "dve"
            evac_rr[0] += 1
        if eng == "act":
            ACT("copy", reads, writes, out=out_ap, in_=in_ap)
        else:
            S.op(eng, "tensor_copy", dict(out=out_ap, in_=in_ap), reads, writes)

    def load_x(s):
        xv = Dm["x"][s].rearrange("(t p) d -> p t d", p=128)
        for g in range(4):
            DMA("sp", [], bX[4 * g:4 * g + 4], semkey=f"dma_X{g}", out=X[:, 4 * g:4 * g + 4, :],
                in_=xv[:, 4 * g:4 * g + 4, :])

    def compute_ada(s, l, half, off):
        condT = aview(off, [128, 8], F32)
        condB = aview(off + 64, [128, 8, 128], F32)
        gtmp = aview(off + 64 + 4096, [128, D], F32)
        wsl = [aview(off + 64 + 4096 + 4096 + i * 16384, [128, 8, 512], F32) for i in range(3)]
        b_cond, b_condB, b_g = Buf("cond"), Buf("condB"), Buf("gtmp")
        b_w = [Buf(f"adaw{i}") for i in range(3)]
        DMA("sp", [], [b_cond], semkey="dma_cond", out=condT, in_=Dm["c"][s].rearrange("(kt p) -> p kt", p=128),
            allow_slow_non_contiguous=True)
        ACT("activation", [b_cond], [b_cond], out=condT, in_=condT, func=AF.Silu)
        DVE("tensor_copy", [b_cond], [b_condB], out=condB, in_=condT.unsqueeze(2).to_broadcast([128, 8, 128]))
        gname = "g_mix" if half == 0 else "g_ffn"
        DMA("sp", [], [b_g], semkey="dma_gtmp", out=gtmp, in_=Dm[gname][l].partition_broadcast(128))
        c_base = half * 3 * D
        DMA("sp", [], [bADA], semkey="dma_ADA", out=ADA[:].rearrange("p a b -> p (a b)"),
            in_=Dm["b_ada"][l, c_base:c_base + 3 * D].partition_broadcast(128))
        def ada_load(j):
            c0 = c_base + j * 512
            DMA("sp", [], [b_w[j % 3]], semkey=f"dma_adaw{j % 3}", out=wsl[j % 3],
                in_=Dm["w_ada"][l, :, c0:c0 + 512].rearrange("(kt p) c -> p kt c", p=128))
        for j in range(3):
            ada_load(j)
        for j in range(6):
            w = wsl[j % 3]
            pb = 6 + (j % 2)
            for kt in range(8):
                PE("matmul", [b_condB, b_w[j % 3]], [bP[pb]], last=(kt == 7), out=pbank[pb][:], lhsT=condB[:, kt, :],
                   rhs=w[:, kt, :], start=(kt == 0), stop=(kt == 7))
            if j + 3 < 6:
                ada_load(j + 3)
            dst = ADA[:, j // 2, (j % 2) * 512:(j % 2) * 512 + 512]
            DVE("tensor_tensor", [bP[pb], bADA], [bADA], out=dst, in0=dst, in1=pbank[pb][:], op=ALU.add)
        DVE("scalar_tensor_tensor", [bADA, b_g], [bADA], out=ADA[:, 1, :], in0=ADA[:, 1, :], scalar=1.0, in1=gtmp,
            op0=ALU.add, op1=ALU.mult)

    stats_state = {"early": False}

    def stats_all(junk, early=False):
        if not early and stats_state["early"]:
            stats_state["early"] = False
            return
        if early:
            stats_state["early"] = True
        POOL("memset", [bSS], [bSS], ap=ss[:], constant=0.0)
        for tt in range(NT):
            ACT("activation", [bX[tt], bSS], [bSS], out=junk, in_=X[:, tt, :], func=AF.Square,
                accum_out=ss[:, tt:tt + 1])
        DVE("tensor_scalar", [bSS], [bSS], out=rstd[:], in0=ss[:], scalar1=1.0 / D, scalar2=EPS,
            op0=ALU.mult, op1=ALU.add)
        ACT("activation", [bSS], [bSS], out=rstd[:], in_=rstd[:], func=AF.Sqrt)
        DVE("reciprocal", [bSS], [bSS], out=rstd[:], in_=rstd[:])

    def norm_tile(tt, hbuf, b_h, junk, b_junk, mul_ap, mul_bufs):
        DVE("scalar_tensor_tensor", [bX[tt], bSS] + mul_bufs, [b_h], out=hbuf, in0=X[:, tt, :],
            scalar=rstd[:, tt:tt + 1], in1=mul_ap, op0=ALU.mult, op1=ALU.mult)

    def transpose_tile(h_bf, b_h, hT, b_hT, c0, pb):
        pv = pbank[pb][:].bitcast(BF16).rearrange("p (a b) -> p a b", a=8)
        for kt in range(8):
            PE("transpose", [b_h, bCONST], [bP[pb]], last=(kt == 7), out=pv[:, kt, :],
               in_=h_bf[:, kt * 128:(kt + 1) * 128], identity=ident_bf[:])
        evac(hT[:, :, c0:c0 + 128], pv, [bP[pb]], [b_hT])

    def zero_ss():
        pass

    def modulated_h_tile(tt, hbuf, b_h, hbf, b_hbf, junk, b_junk):
        norm_tile(tt, hbuf, b_h, junk, b_junk, ADA[:, 1, :], [bADA])
        POOL("tensor_tensor", [b_h, bADA], [b_hbf], out=hbf, in0=hbuf, in1=ADA[:, 0, :], op=ALU.add)

    def range_reduce(dst, src, tmpf, tmpi, bS):
        DVE("tensor_scalar", [bS], [bS], out=tmpf, in0=src, scalar1=1.0 / TWO_PI, scalar2=None, op0=ALU.mult)
        DVE("tensor_copy", [bS], [bS], out=tmpi, in_=tmpf)
        DVE("tensor_copy", [bS], [bS], out=tmpf, in_=tmpi)
        DVE("scalar_tensor_tensor", [bS], [bS], out=dst, in0=tmpf, scalar=-TWO_PI, in1=src, op0=ALU.mult, op1=ALU.add)
        DVE("tensor_scalar", [bS], [bS], out=dst, in0=dst, scalar1=3.1415925, scalar2=-3.1415925, op0=ALU.min,
            op1=ALU.max)

    def setup_ssm(l):
        B = {k: Buf("su_" + k) for k in (
            "lr", "li", "Br", "Bi", "Cs", "dt", "dsk", "xa", "tau", "angT", "xT", "E", "S", "C", "rr", "P", "k", "kt",
            "bb", "t16", "X12", "A12", "CT", "G", "tG", "Wbu", "X1r", "Wall", "KT8", "Wi", "cs8", "tab", "tabt", "cf")}
        G = aview(0, [128, 32, 9, 16], F32)
        WbuT = aview(18432, [128, 32, 8, 16], F32)
        X1rep = aview(34816, [128, 32, 8, 16], F32)
        KT8 = aview(51200, [128, 32, 128], F32)
        Wi = aview(67584, [128, 32, 128], F32)
        Wall = aview(83968, [128, 32, 4, 128], BF16)
        so = [116736]

        def small(shape, dt=F32):
            n = 4
            for s_ in shape[1:]:
                n *= s_
            v = aview(so[0], shape, dt)
            so[0] += n
            return v
        lrT, liT, dtt, xre, ang = [small([128, 32]) for _ in range(5)]
        nre, den, kre, kim, t32a, t32b, C8, S8, cw2 = [small([128, 32]) for _ in range(9)]
        dsk = small([128, 32])
        NTAU = 17
        tauI = small([128, 17], I32)
        tauv = small([128, 17])
        xo = [0]

        def xsmall(shape, dt=F32):
            n = 4
            for s_ in shape[1:]:
                n *= s_
            v = xview(xo[0], shape, dt)
            xo[0] += n
            return v
        Br2, Bi2, CreT2, CimT2, bb_re, bb_im, X1, X2, t16 = [xsmall([128, 32, 16]) for _ in range(9)]
        Csrc_re, Csrc_im = [xsmall([128, 4, 2, 64]) for _ in range(2)]
        tmpG = xsmall([128, 32, 9, 16])
        NTAU = 17
        angT, xT, Et, St, Ct, tmpf = [xsmall([128, 32, NTAU]) for _ in range(6)]
        tmpi = xsmall([128, 32, NTAU], I32)
        P17r, P17i = [xsmall([128, 32, NTAU]) for _ in range(2)]
        A1, A2 = [xsmall([128, 32, 9]) for _ in range(2)]
        P_re, P_im = P17r[:, :, 0:9], P17i[:, :, 0:9]
        PrR, PiR = P17r[:, :, 9:17], P17i[:, :, 9:17]

        for half in range(2):
            ps_ = slice(64 * half, 64 * half + 64)
            DMA("sp", [], [B["lr"]], semkey="dma_su_lr", out=lrT[ps_, :], in_=Dm["lam_re"][l].rearrange("g n -> n g"),
                allow_slow_non_contiguous=True)
            DMA("sp", [], [B["li"]], semkey="dma_su_li", out=liT[ps_, :], in_=Dm["lam_im"][l].rearrange("g n -> n g"),
                allow_slow_non_contiguous=True)
            DMA("sp", [], [B["Br"]], semkey="dma_su_Br", out=Br2[ps_, :, :], in_=Dm["b_re"][l].rearrange("g n h -> n g h"))
            DMA("sp", [], [B["Bi"]], semkey="dma_su_Bi", out=Bi2[ps_, :, :], in_=Dm["b_im"][l].rearrange("g n h -> n g h"))
            DMA("sp", [], [B["Cs"]], semkey="dma_su_Cs", out=Csrc_re[:, :, half, :],
                in_=Dm["c_re"][l].rearrange("g h n -> (g h) n").rearrange("(gt p) n -> p gt n", p=128))
            DMA("sp", [], [B["Cs"]], semkey="dma_su_Cs", out=Csrc_im[:, :, half, :],
                in_=Dm["c_im"][l].rearrange("g h n -> (g h) n").rearrange("(gt p) n -> p gt n", p=128))
        DMA("sp", [], [B["dt"]], semkey="dma_su_dt", out=dtt, in_=Dm["log_dt"][l].partition_broadcast(128))
        for j in range(8):
            DMA("sp", [], [B["dsk"]], semkey="dma_su_dsk", out=dsk[16 * j:16 * j + 16, :],
                in_=Dm["d_skip"][l].rearrange("g h -> h g"), allow_slow_non_contiguous=True)
        POOL("iota", [], [B["tau"]], out=tauI, pattern=[[1, NTAU]], base=0, channel_multiplier=0)
        DVE("tensor_copy", [B["tau"]], [B["tau"]], out=tauv, in_=tauI)
        DVE("tensor_scalar", [B["tau"]], [B["tau"]], out=tauv, in0=tauv, scalar1=-8.0, scalar2=None, op0=ALU.add)
        DVE("tensor_scalar", [B["tau"]], [B["tau"]], out=t32b[:, 0:NTAU], in0=tauv, scalar1=-1.0, scalar2=None, op0=ALU.mult)
        DVE("tensor_tensor", [B["tau"]], [B["tau"]], out=tauv, in0=tauv, in1=t32b[:, 0:NTAU], op=ALU.max)
        DVE("tensor_scalar", [B["tau"]], [B["tau"]], out=tauv, in0=tauv, scalar1=-1.0, scalar2=8.0, op0=ALU.mult,
            op1=ALU.add)
        ACT("activation", [B["dt"]], [B["dt"]], out=dtt, in_=dtt, func=AF.Exp)
        DVE("tensor_tensor", [B["lr"], B["dt"]], [B["xa"]], out=xre, in0=lrT, in1=dtt, op=ALU.mult)
        DVE("tensor_tensor", [B["li"], B["dt"], B["xa"]], [B["xa"]], out=ang, in0=liT, in1=dtt, op=ALU.mult)
        s3n = [128, 32, NTAU]
        tb_ = tauv.unsqueeze(1).to_broadcast(s3n)
        DVE("tensor_tensor", [B["xa"], B["tau"]], [B["angT"]], out=angT, in0=ang.unsqueeze(2).to_broadcast(s3n), in1=tb_,
            op=ALU.mult)
        DVE("tensor_tensor", [B["xa"], B["tau"]], [B["xT"]], out=xT, in0=xre.unsqueeze(2).to_broadcast(s3n), in1=tb_,
            op=ALU.mult)
        ACT("activation", [B["xT"]], [B["E"]], out=Et, in_=xT, func=AF.Exp)
        bR = Buf("su_rr1")
        range_reduce(St, angT, tmpf, tmpi, bR)
        ACT("activation", [bR, B["angT"]], [B["S"]], out=St, in_=St, func=AF.Sin)
        DVE("tensor_scalar", [B["angT"], bR], [B["angT"]], out=angT, in0=angT, scalar1=0.5 * math.pi, scalar2=None,
            op0=ALU.add)
        range_reduce(Ct, angT, tmpf, tmpi, bR)
        ACT("activation", [bR, B["angT"]], [B["C"]], out=Ct, in_=Ct, func=AF.Sin)
        DVE("tensor_tensor", [B["E"], B["C"]], [B["P"]], out=P17r, in0=Et, in1=Ct, op=ALU.mult)
        DVE("tensor_tensor", [B["E"], B["S"], B["P"]], [B["P"]], out=P17i, in0=Et, in1=St, op=ALU.mult)
        DVE("tensor_copy", [B["E"], B["tau"]], [B["cs8"], B["tau"]], out=t32b, in_=Et[:, :, 8])
        DMA("sp", [B["cs8"]], [], semkey="dma_setup_o", out=RHOD[l], in_=t32b)
        rk, wk = [B["P"], B["lr"], B["li"], B["k"], B["kt"]], [B["k"], B["kt"]]
        DVE("tensor_scalar", rk, wk, out=nre, in0=P_re[:, :, 1], scalar1=-1.0, scalar2=None, op0=ALU.add)
        nim = P_im[:, :, 1]
        DVE("tensor_tensor", rk, wk, out=den, in0=lrT, in1=lrT, op=ALU.mult)
        DVE("tensor_tensor", rk, wk, out=t32a, in0=liT, in1=liT, op=ALU.mult)
        DVE("tensor_tensor", rk, wk, out=den, in0=den, in1=t32a, op=ALU.add)
        DVE("reciprocal", rk, wk, out=den, in_=den)
        DVE("tensor_tensor", rk, wk, out=kre, in0=nre, in1=lrT, op=ALU.mult)
        DVE("tensor_tensor", rk, wk, out=t32a, in0=nim, in1=liT, op=ALU.mult)
        DVE("tensor_tensor", rk, wk, out=kre, in0=kre, in1=t32a, op=ALU.add)
        DVE("tensor_tensor", rk, wk, out=kre, in0=kre, in1=den, op=ALU.mult)
        DVE("tensor_tensor", rk, wk, out=kim, in0=nim, in1=lrT, op=ALU.mult)
        DVE("tensor_tensor", rk, wk, out=t32a, in0=nre, in1=liT, op=ALU.mult)
        DVE("tensor_tensor", rk, wk, out=kim, in0=kim, in1=t32a, op=ALU.subtract)
        DVE("tensor_tensor", rk, wk, out=kim, in0=kim, in1=den, op=ALU.mult)
        kre_b = kre.unsqueeze(2).to_broadcast([128, 32, 16])
        kim_b = kim.unsqueeze(2).to_broadcast([128, 32, 16])
        rb_, wb_ = [B["k"], B["Br"], B["Bi"], B["bb"], B["t16"]], [B["bb"], B["t16"]]
        DVE("tensor_tensor", rb_, wb_, out=bb_re, in0=Br2, in1=kre_b, op=ALU.mult)
        DVE("tensor_tensor", rb_, wb_, out=t16, in0=Bi2, in1=kim_b, op=ALU.mult)
        DVE("tensor_tensor", rb_, wb_, out=bb_re, in0=bb_re, in1=t16, op=ALU.subtract)
        DVE("tensor_tensor", rb_, wb_, out=bb_im, in0=Bi2, in1=kre_b, op=ALU.mult)
        DVE("tensor_tensor", rb_, wb_, out=t16, in0=Br2, in1=kim_b, op=ALU.mult)
        DVE("tensor_tensor", rb_, wb_, out=bb_im, in0=bb_im, in1=t16, op=ALU.add)
        lo, hi = slice(0, 64), slice(64, 128)
        rx, wx = [B["bb"], B["X12"]], [B["X12"]]
        DVE("tensor_copy", rx, wx, out=X1[lo], in_=bb_re[lo])
        DVE("tensor_copy", rx, wx, out=X1[hi], in_=bb_im[hi])
        DVE("tensor_scalar", rx, wx, out=X2[lo], in0=bb_im[lo], scalar1=-1.0, scalar2=None, op0=ALU.mult)
        DVE("tensor_copy", rx, wx, out=X2[hi], in_=bb_re[hi])
        ra, wa = [B["P"], B["A12"]], [B["A12"]]
        POOL("tensor_copy", ra, wa, out=A1[lo], in_=P_re[lo])
        POOL("tensor_scalar", ra, wa, out=A1[hi], in0=P_im[hi], scalar1=-1.0, scalar2=None, op0=ALU.mult)
        POOL("tensor_scalar", ra, wa, out=A2[lo], in0=P_im[lo], scalar1=-1.0, scalar2=None, op0=ALU.mult)
        POOL("tensor_scalar", ra, wa, out=A2[hi], in0=P_re[hi], scalar1=-1.0, scalar2=None, op0=ALU.mult)
        for bi_, (src, dst) in enumerate(((Csrc_re, CreT2), (Csrc_im, CimT2))):
            for gt in range(4):
                PE("transpose", [B["Cs"], bCONST], [bP[bi_]], out=pbank[bi_][:, gt * 128:(gt + 1) * 128],
                   in_=src[:, gt, :, :].rearrange("p a b -> p (a b)"), identity=ident_f[:])
            ACT("copy", [bP[bi_], B["CT"]], [B["CT"]], out=dst.rearrange("p g h -> p (g h)"), in_=pbank[bi_][:])
        s4 = [128, 32, 9, 16]
        DVE("tensor_tensor", [B["CT"], B["A12"]], [B["G"]], out=G, in0=CreT2.unsqueeze(2).to_broadcast(s4),
            in1=A1.unsqueeze(3).to_broadcast(s4), op=ALU.mult)
        DVE("tensor_tensor", [B["CT"], B["A12"]], [B["tG"]], out=tmpG, in0=CimT2.unsqueeze(2).to_broadcast(s4),
            in1=A2.unsqueeze(3).to_broadcast(s4), op=ALU.mult)
        DVE("tensor_tensor", [B["G"], B["tG"]], [B["G"]], out=G, in0=G, in1=tmpG, op=ALU.add)
        s4b = [128, 32, 8, 16]
        tmpW = tmpG[:, :, 0:8, :]
        DVE("tensor_tensor", [B["X12"], B["P"]], [B["Wbu"]], out=WbuT, in0=X1.unsqueeze(2).to_broadcast(s4b),
            in1=PrR.unsqueeze(3).to_broadcast(s4b), op=ALU.mult)
        DVE("tensor_tensor", [B["X12"], B["P"], B["G"], B["tG"]], [B["tG"]], out=tmpW, in0=X2.unsqueeze(2).to_broadcast(s4b),
            in1=PiR.unsqueeze(3).to_broadcast(s4b), op=ALU.mult)
        DVE("tensor_tensor", [B["Wbu"], B["tG"]], [B["Wbu"]], out=WbuT, in0=WbuT, in1=tmpW, op=ALU.add)
        POOL("tensor_copy", [B["X12"]], [B["X1r"]], out=X1rep, in_=X1.unsqueeze(2).to_broadcast(s4b))
        POOL("tensor_copy", [B["G"]], [B["Wall"]], out=Wall[:, :, 3, :].rearrange("p g (t h) -> p g t h", t=8),
             in_=G[:, :, 1:9, :])
        for g4 in range(8):
            b1, b2 = 2 + 2 * (g4 % 2), 3 + 2 * (g4 % 2)
            for gl in range(4):
                g = 4 * g4 + gl
                PE("transpose", [B["Wbu"], bCONST], [bP[b1]], last=False, out=pbank[b1][:, gl * 128:(gl + 1) * 128],
                   in_=WbuT[:, g, :, :].rearrange("p a b -> p (a b)"), identity=ident_f[:])
                PE("matmul", [B["X1r"], B["G"]], [bP[b2]], last=(gl == 3), out=pbank[b2][:, gl * 128:(gl + 1) * 128],
                   lhsT=X1rep[:, g, :, :].rearrange("p a b -> p (a b)"),
                   rhs=G[:, g, 0:8, :].rearrange("p a b -> p (a b)"), start=True, stop=True)
            ACT("copy", [bP[b1], B["Wall"]], [B["Wall"]], out=Wall[:, 4 * g4:4 * g4 + 4, 1, :],
                in_=pbank[b1][:].rearrange("p (g c) -> p g c", g=4))
            DVE("tensor_copy", [bP[b2], B["KT8"]], [B["KT8"]], out=KT8[:, 4 * g4:4 * g4 + 4, :],
                in_=pbank[b2][:].rearrange("p (g c) -> p g c", g=4))
        POOL("tensor_copy", [B["Wall"]], [B["Wall"]], out=Wall[:, :, 2, 0:64], in_=Wall[:, :, 1, 64:128])
        POOL("tensor_scalar", [B["Wall"]], [B["Wall"]], out=Wall[:, :, 2, 64:128], in0=Wall[:, :, 1, 0:64], scalar1=-1.0,
             scalar2=None, op0=ALU.mult)
        POOL("memset", [], [B["Wi"]], ap=Wi, constant=0.0)
        for j in range(8):
            DVE("scalar_tensor_tensor", [B["KT8"], B["Wi"], bCONST], [B["Wi"]], out=Wi[:, :, 16 * j:128],
                in0=KT8[:, :, 0:128 - 16 * j], scalar=jmask[:, j:j + 1], in1=Wi[:, :, 16 * j:128], op0=ALU.mult, op1=ALU.add)
        s3 = [128, 32, 128]
        DVE("tensor_tensor", [B["KT8"], B["Wi"], B["dsk"], bCONST], [B["KT8"]], out=KT8,
            in0=ident_f[:].unsqueeze(1).to_broadcast(s3), in1=dsk.unsqueeze(2).to_broadcast(s3), op=ALU.mult)
        DVE("tensor_tensor", [B["Wi"], B["KT8"], B["Wall"]], [B["Wall"]], out=Wall[:, :, 0, :], in0=Wi, in1=KT8, op=ALU.add)
        DMA("sp", [B["Wall"]], [], semkey="dma_setup_o", out=WSSM[l], in_=Wall)
        if debug and stop == "S":
            allb = list(B.values())
            dbg("Wall", Wall, [128, 32, 4, 128], BF16, allb)
            dbg("G", G, [128, 32, 9, 16], F32, allb)
            dbg("P_re", P_re, [128, 32, 9], F32, allb)
            dbg("P_im", P_im, [128, 32, 9], F32, allb)
            dbg("X1", X1, [128, 32, 16], F32, allb)
        DVE("tensor_scalar", [B["xa"], B["k"], B["kt"]], [B["kt"]], out=t32a, in0=ang, scalar1=8.0, scalar2=None, op0=ALU.mult)
        bR2 = Buf("su_rr2")
        DVE("tensor_copy", [B["kt"]], [bR2], out=cw2, in_=t32a)
        range_reduce(C8, cw2, S8, nre.bitcast(I32), bR2)
        S.barrier()
        bT = Buf("su_tab")
        rS, wS = [bT], [bT]
        Tc = aview(0, [128, 32, 256], F32)
        Ts = aview(32768, [128, 32, 256], F32)
        tbf = aview(65536, [128, 32, 256], F32)
        tbi = xview(0, [128, 32, 256], I32)
        cI = xview(32768, [128, 256], I32)
        cF = xview(32768 + 1024, [128, 256], F32)
        POOL("iota", rS, wS, out=cI, pattern=[[1, 256]], base=0, channel_multiplier=0)
        DVE("tensor_copy", rS, wS, out=cF, in_=cI)
        s3t = [128, 32, 256]
        DVE("tensor_tensor", rS, wS, out=Tc, in0=C8.unsqueeze(2).to_broadcast(s3t), in1=cF.unsqueeze(1).to_broadcast(s3t),
            op=ALU.mult)
        range_reduce(Ts, Tc, tbf, tbi, bT)
        ACT("activation", rS, wS, out=Ts, in_=Ts, func=AF.Sin)
        DMA("sp", rS, [], semkey="dma_setup_o", out=TAB[l, :, :, 1, :], in_=Ts)
        DVE("tensor_scalar", rS, wS, out=Tc, in0=Tc, scalar1=0.5 * math.pi, scalar2=None, op0=ALU.add)
        range_reduce(Tc, Tc, tbf, tbi, bT)
        ACT("activation", rS, wS, out=Tc, in_=Tc, func=AF.Sin)
        DMA("sp", rS, [], semkey="dma_setup_o", out=TAB[l, :, :, 0, :], in_=Tc)
        if debug and stop == "S":
            dbg("Tc", Tc, [128, 32, 256], F32, rS)
            dbg("Ts", Ts, [128, 32, 256], F32, rS)
        S.barrier()

    if do_setup:
        for l in range(n_layers):
            setup_ssm(l)


    b_conv = {nm: [Buf(f"cv_{nm}{e}") for e in range(NE)] for nm in ("w1", "w3", "w2")}
    conv_jobs = [(nm, e) for e in range(NE) for nm in ("w1", "w3", "w2")] if (sparse and n_layers > 1) else []
    conv_dst = {"w1": W1B, "w3": W3B, "w2": W2B}

    def issue_conv(n):
        for _ in range(min(n, len(conv_jobs))):
            nm, e = conv_jobs.pop(0)
            src = Dm[f"{nm}_moe"][0, e]
            dst = conv_dst[nm][e]
            rows = src.shape[0]
            hr = rows // 2
            for (a, b) in ((0, hr), (hr, rows)):
                DMA("pool", [], [b_conv[nm][e]], semkey=f"dma_cv_{nm}{e}", out=dst[a:b, :], in_=src[a:b, :])

    def sparse_moe(s, l):
        m = l // 2
        issue_conv(len(conv_jobs))
        h2T = aview(0, [128, 8, T], BF16)
        h2tok = aview(32768, [128, NT, D], BF16)
        hbuf2 = [aview(65536, [128, D], F32), aview(73728, [128, D], F32)]
        b_h2 = [Buf("h0"), Buf("h1")]
        junk = aview(69632, [128, D], BF16)
        sm = [118784]

        def small(shape, dt=F32):
            n = 2 if dt == BF16 else 4
            for s_ in shape[1:]:
                n *= s_
            n = (n + 3) // 4 * 4
            v = aview(sm[0], shape, dt)
            sm[0] += n
            return v
        s3 = [128, NT, 8]
        WR = small([128, 8, 8], BF16)
        logits, gates, sel, eq1, l2, Wn, TOT, PSa, PSb, val = [small(s3) for _ in range(10)]
        m1, m2, d1, d2, g1, g2 = [small([128, NT]) for _ in range(6)]
        d1i, d2i = small([128, NT], I32), small([128, NT], I32)
        brt, rowb, nev = small([128, 8]), small([128, 8]), small([128, 8])
        rowbi = small([128, 8], I32)
        thr = small([128, 4])
        thri = small([128, 4], I32)
        flagF = small([128, 8, 4])
        flagI = small([128, 8, 4], I32)
        selbf = small([128, 128], BF16)
        Ltri = small([128, 128], BF16)
        ones128 = small([128, 128], BF16)
        b_h2T, b_h, b_junk = Buf("h2T"), Buf("h"), Buf("junk")
        b_tok = [Buf(f"h2tok{i}") for i in range(NT)]
        b_WR, b_rt, b_cst, b_flag, b_HS, b_RS = Buf("WR"), Buf("router"), Buf("rcst"), Buf("flags"), Buf("HS"), Buf("RS")
        stats_all(junk)
        for tt in range(NT):
            modulated_h_tile(tt, hbuf2[tt % 2], b_h2[tt % 2], h2tok[:, tt, :], b_tok[tt], junk, b_junk)
            transpose_tile(h2tok[:, tt, :], b_tok[tt], h2T, b_h2T, tt * 128, 6 + (tt % 2))
        POOL("memset", [], [b_cst], ap=Ltri, constant=1.0)
        POOL("affine_select", [b_cst], [b_cst], out=Ltri, in_=Ltri, pattern=[[1, 128]], compare_op=ALU.is_gt, fill=0.0,
             base=0, channel_multiplier=-1)
        POOL("memset", [b_cst], [b_cst], ap=ones128, constant=1.0)
        POOL("iota", [b_cst], [b_cst], out=rowbi, pattern=[[T, 8]], base=0, channel_multiplier=0)
        POOL("iota", [b_cst], [b_cst], out=thri, pattern=[[512, 4]], base=0, channel_multiplier=0)
        DVE("tensor_copy", [b_cst], [b_cst], out=rowb, in_=rowbi)
        DVE("tensor_copy", [b_cst], [b_cst], out=thr, in_=thri)
        DMA("pool", [], [b_WR], semkey="dma_WR", out=WR, in_=Dm["w_router"][m].rearrange("(kt p) e -> p kt e", p=128))
        DMA("sp", [], [b_rt], semkey="dma_brt", out=brt, in_=Dm["b_router"][m].partition_broadcast(128))
        pv = pbank[0][:].rearrange("p (a b) -> p a b", a=NT)[:, :, 0:8]
        for tt in range(NT):
            for kt in range(8):
                PE("matmul", [b_WR, b_h2T], [bP[0]], last=(kt == 7 and tt == NT - 1), out=pv[:, tt, :],
                   lhsT=h2T[:, kt, tt * 128:(tt + 1) * 128], rhs=WR[:, kt, :], start=(kt == 0), stop=(kt == 7))
        rR, wR = [b_rt, b_cst], [b_rt]
        DVE("tensor_tensor", [bP[0], b_rt], wR, out=logits, in0=pv, in1=brt.unsqueeze(1).to_broadcast(s3), op=ALU.add)
        DVE("tensor_reduce", rR, wR, out=m1, in_=logits, axis=AX.X, op=ALU.max)
        DVE("tensor_tensor", rR, wR, out=eq1, in0=logits, in1=m1.unsqueeze(2).to_broadcast(s3), op=ALU.is_equal)
        DVE("scalar_tensor_tensor", rR, wR, out=l2, in0=eq1, scalar=-1.0e30, in1=logits, op0=ALU.mult, op1=ALU.add)
        DVE("tensor_reduce", rR, wR, out=m2, in_=l2, axis=AX.X, op=ALU.max)
        DVE("tensor_tensor", rR, wR, out=sel, in0=logits, in1=m2.unsqueeze(2).to_broadcast(s3), op=ALU.is_ge)
        DVE("tensor_tensor", rR, wR, out=l2, in0=logits, in1=m1.unsqueeze(2).to_broadcast(s3), op=ALU.subtract)
        ACT("activation", rR, wR, out=l2, in_=l2, func=AF.Exp)
        DVE("tensor_tensor", rR, wR, out=l2, in0=l2, in1=sel, op=ALU.mult)
        DVE("tensor_reduce", rR, wR, out=m2, in_=l2, axis=AX.X, op=ALU.add)
        DVE("reciprocal", rR, wR, out=m2, in_=m2)
        DVE("tensor_tensor", rR, wR, out=gates, in0=l2, in1=m2.unsqueeze(2).to_broadcast(s3), op=ALU.mult)
        DVE("tensor_copy", rR, wR, out=selbf, in_=sel.rearrange("p a b -> p (a b)"))
        PE("matmul", rR, [bP[1]], last=False, out=pbank[1][:, 0:128], lhsT=Ltri, rhs=selbf, start=True, stop=True)
        PE("matmul", rR, [bP[1]], last=True, out=pbank[1][:, 128:256], lhsT=ones128, rhs=selbf, start=True, stop=True)
        DVE("tensor_copy", [bP[1]] + rR, wR, out=Wn.rearrange("p a b -> p (a b)"), in_=pbank[1][:, 0:128])
        DVE("tensor_copy", [bP[1]] + rR, wR, out=TOT.rearrange("p a b -> p (a b)"), in_=pbank[1][:, 128:256])
        DVE("tensor_copy", rR, wR, out=PSa, in_=TOT)
        src_, dst_ = PSa, PSb
        for d_ in (1, 2, 4, 8):
            DVE("tensor_tensor", rR, wR, out=dst_[:, d_:, :], in0=src_[:, d_:, :], in1=src_[:, 0:NT - d_, :], op=ALU.add)
            DVE("tensor_copy", rR, wR, out=dst_[:, 0:d_, :], in_=src_[:, 0:d_, :])
            src_, dst_ = dst_, src_
        INC = src_
        DVE("tensor_copy", rR, wR, out=nev, in_=INC[:, NT - 1, :])
        DVE("tensor_tensor", rR, wR, out=val, in0=INC, in1=TOT, op=ALU.subtract)
        DVE("tensor_tensor", rR, wR, out=val, in0=val, in1=Wn, op=ALU.add)
        DVE("tensor_tensor", rR, wR, out=val, in0=val, in1=rowb.unsqueeze(1).to_broadcast(s3), op=ALU.add)
        DVE("tensor_tensor", rR, wR, out=l2, in0=val, in1=eq1, op=ALU.mult)
        DVE("tensor_reduce", rR, wR, out=d1, in_=l2, axis=AX.X, op=ALU.add)
        DVE("tensor_tensor", rR, wR, out=l2, in0=gates, in1=eq1, op=ALU.mult)
        DVE("tensor_reduce", rR, wR, out=g1, in_=l2, axis=AX.X, op=ALU.add)
        DVE("tensor_tensor", rR, wR, out=eq1, in0=sel, in1=eq1, op=ALU.subtract)
        DVE("tensor_tensor", rR, wR, out=l2, in0=val, in1=eq1, op=ALU.mult)
        DVE("tensor_reduce", rR, wR, out=d2, in_=l2, axis=AX.X, op=ALU.add)
        DVE("tensor_tensor", rR, wR, out=l2, in0=gates, in1=eq1, op=ALU.mult)
        DVE("tensor_reduce", rR, wR, out=g2, in_=l2, axis=AX.X, op=ALU.add)
        DVE("tensor_copy", rR, wR, out=d1i, in_=d1)
        DVE("tensor_copy", rR, wR, out=d2i, in_=d2)
        DVE("tensor_tensor", rR, [b_flag], out=flagF, in0=nev.unsqueeze(2).to_broadcast([128, 8, 4]),
            in1=thr.unsqueeze(1).to_broadcast([128, 8, 4]), op=ALU.is_gt)
        DVE("tensor_copy", [b_flag], [b_flag], out=flagI, in_=flagF)
        for tt in range(NT):
            for di in (d1i, d2i):
                b_HS = Buf("HS")
                S.dma("pool", dict(out=HS, out_offset=bass.IndirectOffsetOnAxis(ap=di[:, tt:tt + 1], axis=0),
                                   in_=h2tok[:, tt, :], in_offset=None),
                      [b_tok[tt], b_rt], [b_HS], semkey="dma_HS", method="indirect_dma_start")
        if debug and stop == "R":
            dbg("gates", gates, [128, NT, 8], F32, [b_rt])
            dbg("d1", d1, [128, NT], F32, [b_rt])
            dbg("d2", d2, [128, NT], F32, [b_rt])
            dbg("nev", nev, [128, 8], F32, [b_rt])
            dbg("flagI", flagI, [128, 8, 4], I32, [b_flag])
            return
        S.barrier(exclude=())
        actT = aview(0, [128, NFT, 512], BF16)
        W2 = aview(32768, [128, NFT, D], BF16)
        W1S = [aview(77824 + i * 4096, [128, 8, 256], BF16) for i in range(2)]
        W3S = [aview(77824 + 8192 + i * 4096, [128, 8, 256], BF16) for i in range(2)]
        h2Tc = aview(94208, [128, 8, 512], BF16)
        outs2 = [aview(102400 + i * 4096, [128, D], F32) for i in range(2)]
        HSc = aview(110592, [128, 4, D], BF16)
        sil = [aview(22528 + i * 2048, [128, 512], F32) for i in range(2)]
        b_act = [Buf(f"act{i}") for i in range(NFT)]
        b_W2 = [Buf(f"W2g{i}") for i in range(4)]
        b_W1S, b_W3S = [Buf(f"W1S{i}") for i in range(2)], [Buf(f"W3S{i}") for i in range(2)]
        b_outs2 = [Buf("outs0"), Buf("outs1")]
        b_HSc, b_h2Tc, b_sil = Buf("HSc"), Buf("h2Tc"), [Buf("sil0"), Buf("sil1")]
        st_it = 0
        slab = 0
        pe_it = 0
        ev_it = 0
        for e in range(NE):
            w1d, w3d, w2d = W1B[e], W3B[e], W2B[e]
            for c in range(4):
                r0 = e * T + c * 512
                S.cond_begin(flagI[0:1, e, c:c + 1], [b_flag])
                DMA("sp", [b_HS], [b_HSc], semkey="dma_HSc", out=HSc,
                    in_=HS[r0:r0 + 512, :].rearrange("(t p) d -> p t d", p=128))
                for gi, (f0, nf) in enumerate(FGROUPS):
                    DMA("pool", [b_conv["w2"][e]], [b_W2[gi]], semkey=f"dma_W2g{gi}", out=W2[:, f0:f0 + nf, :],
                        in_=w2d[f0 * 128:(f0 + nf) * 128, :].rearrange("(ft p) d -> p ft d", p=128))
                for t4 in range(4):
                    transpose_tile(HSc[:, t4, :], b_HSc, h2Tc, b_h2Tc, t4 * 128, 6 + (t4 % 2))
                for ft in range(NFT):
                    if ft % 2 == 0:
                        si = slab % 2
                        slab += 1
                        DMA("sp", [b_conv["w1"][e]], [b_W1S[si]], semkey=f"dma_W1S{si}", out=W1S[si],
                            in_=w1d[:, ft * 128:(ft + 2) * 128].rearrange("(kt p) c -> p kt c", p=128))
                        DMA("sp", [b_conv["w3"][e]], [b_W3S[si]], semkey=f"dma_W3S{si}", out=W3S[si],
                            in_=w3d[:, ft * 128:(ft + 2) * 128].rearrange("(kt p) c -> p kt c", p=128))
                    fsl = slice((ft % 2) * 128, (ft % 2) * 128 + 128)
                    pa, pb_ = (pe_it % 2) * 2, (pe_it % 2) * 2 + 1
                    sl_, b_sl = sil[pe_it % 2], b_sil[pe_it % 2]
                    pe_it += 1
                    for kt in range(8):
                        PE("matmul", [b_W1S[si], b_h2Tc], [bP[pa]], last=(kt == 7), out=pbank[pa][:],
                           lhsT=W1S[si][:, kt, fsl], rhs=h2Tc[:, kt, :], start=(kt == 0), stop=(kt == 7))
                    for kt in range(8):
                        PE("matmul", [b_W3S[si], b_h2Tc], [bP[pb_]], last=(kt == 7), out=pbank[pb_][:],
                           lhsT=W3S[si][:, kt, fsl], rhs=h2Tc[:, kt, :], start=(kt == 0), stop=(kt == 7))
                    ACT("activation", [bP[pa]], [b_sl], out=sl_, in_=pbank[pa][:], func=AF.Silu)
                    DVE("tensor_tensor", [b_sl, bP[pb_]], [b_act[ft]], out=actT[:, ft, :], in0=sl_, in1=pbank[pb_][:],
                        op=ALU.mult)
                for st_ in range(4):
                    ob2, b_ob2 = outs2[st_it % 2], b_outs2[st_it % 2]
                    for half in range(2):
                        bk = 4 + (ev_it % 4)
                        ev_it += 1
                        hsl = slice(half * 512, (half + 1) * 512)
                        for ft in range(NFT):
                            gi = 0 if ft < 6 else (1 if ft < 12 else (2 if ft < 17 else 3))
                            PE("matmul", [b_act[ft], b_W2[gi]], [bP[bk]], last=(ft == NFT - 1), out=pbank[bk][:],
                               lhsT=actT[:, ft, st_ * 128:(st_ + 1) * 128], rhs=W2[:, ft, hsl], start=(ft == 0),
                               stop=(ft == NFT - 1))
                        evac(ob2[:, hsl], pbank[bk][:], [bP[bk]], [b_ob2])
                    DMA("pool", [b_ob2], [b_RS], semkey=f"dma_RS{st_it % 2}",
                        out=RS[r0 + st_ * 128:r0 + (st_ + 1) * 128, :], in_=ob2)
                    st_it += 1
                S.cond_end()
        S.barrier()
        r1 = [aview(i * 4096, [128, D], F32) for i in range(2)]
        r2 = [aview(8192 + i * 4096, [128, D], F32) for i in range(2)]
        b_r1, b_r2 = [Buf("r10"), Buf("r11")], [Buf("r20"), Buf("r21")]
        for tt in range(NT):
            i2 = tt % 2
            S.dma("pool", dict(out=r1[i2], out_offset=None, in_=RS,
                               in_offset=bass.IndirectOffsetOnAxis(ap=d1i[:, tt:tt + 1], axis=0)),
                  [b_RS, b_rt], [b_r1[i2]], semkey=f"dma_r1{i2}", method="indirect_dma_start")
            S.dma("pool", dict(out=r2[i2], out_offset=None, in_=RS,
                               in_offset=bass.IndirectOffsetOnAxis(ap=d2i[:, tt:tt + 1], axis=0)),
                  [b_RS, b_rt], [b_r2[i2]], semkey=f"dma_r2{i2}", method="indirect_dma_start")
            DVE("tensor_scalar", [b_r1[i2], b_rt], [b_r1[i2]], out=r1[i2], in0=r1[i2], scalar1=g1[:, tt:tt + 1],
                scalar2=None, op0=ALU.mult)
            DVE("scalar_tensor_tensor", [b_r1[i2], b_r2[i2], b_rt], [b_r1[i2]], out=r1[i2], in0=r2[i2],
                scalar=g2[:, tt:tt + 1], in1=r1[i2], op0=ALU.mult, op1=ALU.add)
            DVE("tensor_tensor", [b_r1[i2], bADA], [b_r1[i2]], out=r1[i2], in0=r1[i2], in1=ADA[:, 2, :], op=ALU.mult)
            DVE("tensor_tensor", [b_r1[i2], bX[tt]], [bX[tt]], out=X[:, tt, :], in0=X[:, tt, :], in1=r1[i2], op=ALU.add)

    O_WIN = 0
    O_ATT = 0
    O_GT = 16384
    O_U8 = 32896
    O_QT = O_U8 + 16384
    O_KT = O_QT + 16384
    O_V = O_KT + 16384
    O_CS = O_V + 16384
    O_S6 = O_CS + 8192
    O_S7 = O_S6 + 8192

    done = (debug and stop == "S")
    for s in range(n_seq):
        if done:
            break
        load_x(s)
        for l in range(n_layers):
            WIN = aview(O_WIN, [128, 8, 2056], BF16)
            b_WIN = Buf("WIN")
            for g in range(4):
                DMA("pool", [], [b_WIN], semkey="dma_WIN", out=WIN[:, 2 * g:2 * g + 2, :],
                    in_=Dm["w_in"][l, 256 * g:256 * g + 256, 0:2056].rearrange("(kt p) c -> p kt c", p=128))
            stats_all(aview(O_CS, [128, D], BF16), early=True)
            compute_ada(s, l, 0, O_U8)
            issue_conv(3)
            S.barrier()
            U8 = aview(O_U8, [128, 2, 32, 8, 16], BF16)
            qT = aview(O_QT, [128, 4, T], BF16)
            kT = aview(O_KT, [128, 4, T], BF16)
            V = aview(O_V, [128, NT, 512], BF16)
            cs = aview(O_CS, [8, T], F32)
            hTc = aview(O_S6, [128, 8, 512], BF16)
            hbuf = aview(O_S7, [128, D], F32)
            hbf2 = [aview(O_S7 + 4096, [128, D], BF16), aview(O_S7 + 6144, [128, D], BF16)]
            junk = aview(O_S7 + 8192, [128, D], BF16)
            etmp = aview(O_S7 + 10240, [8, 512], F32)
            ltmp = aview(O_S7 + 12288, [8, 512], F32)
            negb = aview(O_S7 + 14336, [8, 1], F32)
            b_hbf2 = [Buf("hbf0"), Buf("hbf1")]
            stats_all(junk)
            b_U8, b_cs, b_hTc = [Buf("U8a"), Buf("U8b")], Buf("cs"), Buf("hTc")
            b_qT = [Buf(f"qT{i}") for i in range(4)]
            b_kT = [Buf(f"kT{i}") for i in range(4)]
            b_V = [Buf(f"V{i}") for i in range(NT)]
            b_h, b_hbf, b_junk, b_e, b_l, b_negb = Buf("h"), Buf("hbf"), Buf("junk"), Buf("e"), Buf("l"), Buf("negb")
            DMA("sp", [], [b_negb], semkey="dma_negb", out=negb, in_=Dm["b_forget"][l].rearrange("(h o) -> h o", o=1),
                allow_slow_non_contiguous=True)
            DVE("tensor_scalar", [b_negb], [b_negb], out=negb, in0=negb, scalar1=-1.0, scalar2=None, op0=ALU.mult)
            for c in range(4):
                for t4 in range(4):
                    tt = 4 * c + t4
                    modulated_h_tile(tt, hbuf, b_h, hbf2[tt % 2], b_hbf2[tt % 2], junk, b_junk)
                    transpose_tile(hbf2[tt % 2], b_hbf2[tt % 2], hTc, b_hTc, t4 * 128, 4 + (t4 % 2))
                cols = slice(c * 512, (c + 1) * 512)
                for which, dstT, b_dst, cbase in ((0, qT, b_qT, 0), (1, kT, b_kT, 512)):
                    for ft in range(4):
                        pb = ft
                        for kt in range(8):
                            PE("matmul", [b_WIN, b_hTc], [bP[pb]], last=(kt == 7), out=pbank[pb][:],
                               lhsT=WIN[:, kt, cbase + ft * 128:cbase + ft * 128 + 128], rhs=hTc[:, kt, :],
                               start=(kt == 0), stop=(kt == 7))
                        evac(dstT[:, ft, cols], pbank[pb][:], [bP[pb]], [b_dst[ft]])
                for t4 in range(4):
                    pb = t4
                    for kt in range(8):
                        PE("matmul", [b_WIN, b_hTc], [bP[pb]], last=(kt == 7), out=pbank[pb][:],
                           lhsT=hTc[:, kt, t4 * 128:(t4 + 1) * 128], rhs=WIN[:, kt, 1024:1536],
                           start=(kt == 0), stop=(kt == 7))
                    evac(V[:, 4 * c + t4, :], pbank[pb][:], [bP[pb]], [b_V[4 * c + t4]])
                for kt in range(8):
                    PE("matmul", [b_WIN, b_hTc], [bP[0]], last=(kt == 7), out=pbank[0][0:8, :],
                       lhsT=WIN[:, kt, 1536:1544], rhs=hTc[:, kt, :], start=(kt == 0), stop=(kt == 7))
                ACT("activation", [bP[0], b_negb], [b_e], out=etmp, in_=pbank[0][0:8, :], func=AF.Exp,
                    bias=negb[:, 0:1], scale=-1.0)
                ACT("activation", [b_e], [b_l], out=ltmp, in_=etmp, func=AF.Ln, bias=1.0, scale=1.0)
                init = 0.0 if c == 0 else cs[:, c * 512 - 1:c * 512]
                DVE("tensor_tensor_scan", [b_l, b_cs, bCONST], [b_cs], out=cs[:, c * 512:(c + 1) * 512],
                    data0=ones_f[0:8, 0:1].to_broadcast([8, 512]), data1=ltmp, initial=init, op0=ALU.mult, op1=ALU.add)
                half, po = c // 2, (c % 2) * 64
                for j in range(8):
                    pb = 1 + (j % 3)
                    for kt in range(8):
                        PE("matmul", [b_WIN, b_hTc], [bP[pb]], last=(kt == 7), out=pbank[pb][po:po + 64, :],
                           lhsT=hTc[:, kt, j::8], rhs=WIN[:, kt, 1544:2056], start=(kt == 0), stop=(kt == 7),
                           tile_position=(0, po))
                    evac(U8[po:po + 64, half, :, j, :], pbank[pb][po:po + 64, :].rearrange("p (g h) -> p g h", g=32),
                         [bP[pb]], [b_U8[half]])
            if debug and stop == "A":
                dbg("qT", qT, [128, 4, T], BF16, b_qT)
                dbg("kT", kT, [128, 4, T], BF16, b_kT)
                dbg("V", V, [128, NT, 512], BF16, b_V)
                dbg("U8", U8, [128, 2, 32, 8, 16], BF16, b_U8)
                dbg("cs", cs, [8, T], F32, [b_cs])
                dbg("ADA", ADA[:], [128, 3, D], F32, [bADA])
                done = True
                break
            S.barrier()
            issue_conv(4)
            attT = aview(O_ATT, [128, 4, T], BF16)
            b_attT = [Buf(f"attT{i}") for i in range(4)]
            Fs = aview(O_S6, [128, 3, T], BF16)
            O_B7 = O_S6 + 12288
            PT = [[aview(O_B7 + (hh * 3 + bf) * 1024, [128, 512], BF16) for bf in range(3)] for hh in range(2)]
            recip = [aview(O_B7 + 6144 + i * 2048, [128, 512], F32) for i in range(2)]
            cs_tok = aview(O_B7 + 10240, [128, NT, 8], F32)
            b_PT = [[Buf(f"PT{hh}{bf}") for bf in range(3)] for hh in range(2)]
            b_recip = [Buf("recip0"), Buf("recip1")]
            b_cstok, b_Fs = Buf("cs_tok"), Buf("Fs")
            hi = aview(O_GT, [8, T], BF16)
            mid = aview(O_GT + 4096, [8, T], BF16)
            lo_ = aview(O_GT + 8192, [8, T], BF16)
            r1 = aview(O_GT + 12288, [8, 512], F32)
            r2 = aview(O_GT + 14336, [8, 512], F32)
            b_split, b_r = Buf("split"), Buf("r12")
            if not (debug and stop in ("C", "Conly")):
                POOL("memset", [], [b_Fs], ap=Fs, constant=0.0)
                for c in range(4):
                    cl = slice(c * 512, (c + 1) * 512)
                    DVE("tensor_copy", [b_cs], [b_split], out=hi[:, cl], in_=cs[:, cl])
                    DVE("tensor_tensor", [b_cs, b_split], [b_r], out=r1, in0=cs[:, cl], in1=hi[:, cl], op=ALU.subtract)
                    DVE("tensor_copy", [b_r], [b_split], out=mid[:, cl], in_=r1)
                    DVE("tensor_tensor", [b_r, b_split], [b_r], out=r2, in0=r1, in1=mid[:, cl], op=ALU.subtract)
                    DVE("tensor_copy", [b_r], [b_split], out=lo_[:, cl], in_=r2)
                for h in range(H):
                    for i, piece in enumerate((hi, mid, lo_)):
                        p0 = 32 * (h % 3) + i
                        DMA("sp", [b_split], [b_Fs], semkey="dma_Fs", out=Fs[p0:p0 + 1, h // 3, :], in_=piece[h:h + 1, :])
                pv = pbank[0][:].rearrange("p (a b) -> p a b", a=NT)[:, :, 0:8]
                for tt in range(NT):
                    PE("transpose", [b_cs, bCONST], [bP[0]], last=(tt == NT - 1), out=pv[:, tt, :],
                       in_=cs[:, tt * 128:(tt + 1) * 128], identity=ident_f[0:8, 0:8])
                evac(cs_tok, pv, [bP[0]], [b_cstok], eng="dve")
                steps = []
                grp = 0
                for hp in range(4):
                    for Q in range(4):
                        for kb in range(4 * Q + 4):
                            steps.append((hp, Q, kb, grp, 4 * Q + 4))
                        grp += 1

                def stage1(i):
                    hp, Q, kb, g_, nkb = steps[i]
                    r = kb - 4 * Q
                    c0 = max(0, r) * 128
                    for hh in range(2):
                        sp_i = 3 * hh + (i % 3)
                        hs = slice(64 * hh, 64 * hh + 64)
                        PE("matmul", [b_kT[hp], b_qT[hp]], [bP[sp_i]], last=False, out=pbank[sp_i][:, c0:512],
                           lhsT=kT[hs, hp, kb * 128:(kb + 1) * 128], rhs=qT[hs, hp, Q * 512 + c0:(Q + 1) * 512],
                           start=True, stop=False)
                    for hh in range(2):
                        h = 2 * hp + hh
                        sp_i = 3 * hh + (i % 3)
                        fp0 = 32 * (h % 3)
                        PE("matmul", [b_Fs, bCONST], [bP[sp_i]], last=(r < 0 and hh == 1), out=pbank[sp_i][:, c0:512],
                           lhsT=neg8[fp0:fp0 + 3, :], rhs=Fs[fp0:fp0 + 3, h // 3, Q * 512 + c0:(Q + 1) * 512],
                           start=False, stop=True)
                    if r >= 0:
                        for hh in range(2):
                            sp_i = 3 * hh + (i % 3)
                            PE("matmul", [bCONST], [bP[sp_i]], last=(hh == 1), out=pbank[sp_i][:, c0:c0 + 128],
                               lhsT=ident_bf[:], rhs=maskneg[:], start=False, stop=True)
                    for hh in range(2):
                        h = 2 * hp + hh
                        sp_i = 3 * hh + (i % 3)
                        ACT("activation", [bP[sp_i], b_cstok], [b_PT[hh][i % 3]], out=PT[hh][i % 3][:, c0:512],
                            in_=pbank[sp_i][:, c0:512], func=AF.Exp, bias=cs_tok[:, kb, h:h + 1], scale=0.125)

                def stage2(i):
                    hp, Q, kb, g_, nkb = steps[i]
                    r = kb - 4 * Q
                    c0 = max(0, r) * 128
                    ob = 6
                    for hh in range(2):
                        h = 2 * hp + hh
                        hs = slice(64 * hh, 64 * hh + 64)
                        PE("matmul", [b_V[kb], b_PT[hh][i % 3]], [bP[ob]], last=False, out=pbank[ob][hs, c0:512],
                           lhsT=V[:, kb, h * 64:(h + 1) * 64], rhs=PT[hh][i % 3][:, c0:512], start=(kb == 0),
                           stop=(kb == nkb - 1), tile_position=(0, 64 * hh))
                    for hh in range(2):
                        hs = slice(64 * hh, 64 * hh + 64)
                        PE("matmul", [bCONST, b_PT[hh][i % 3]], [bP[ob + 1]], last=(hh == 1), out=pbank[ob + 1][hs, c0:512],
                           lhsT=ones_bf[:, 0:64], rhs=PT[hh][i % 3][:, c0:512], start=(kb == 0), stop=(kb == nkb - 1),
                           tile_position=(0, 64 * hh))
                    if kb == nkb - 1:
                        rc, b_rc = recip[g_ % 2], b_recip[g_ % 2]
                        DVE("reciprocal", [bP[ob + 1]], [b_rc], out=rc, in_=pbank[ob + 1][:])
                        DVE("tensor_tensor", [bP[ob], b_rc], [b_attT[hp]], out=attT[:, hp, Q * 512:(Q + 1) * 512],
                            in0=pbank[ob][:], in1=rc, op=ALU.mult)

                for w_ in range(24):
                    PE("matmul", [b_kT[0], b_qT[0]], [bP[6]], last=(w_ == 23), out=pbank[6][:], lhsT=kT[:, 0, 0:128],
                       rhs=qT[:, 0, 0:512], start=True, stop=True)
                stage1(0)
                stage1(1)
                for i in range(len(steps)):
                    if i + 2 < len(steps):
                        stage1(i + 2)
                    stage2(i)
            if debug and stop == "B":
                dbg("attT", attT, [128, 4, T], BF16, b_attT)
                done = True
                break
            S.barrier()
            issue_conv(3)
            gT = aview(O_GT, [128, 4, T], BF16)
            b_gT = [Buf(f"gT{i}") for i in range(4)]
            oc = [O_QT]

            def calloc(shape, dt):
                n = 2 if dt == BF16 else 4
                for s_ in shape[1:]:
                    n *= s_
                v = aview(oc[0], shape, dt)
                oc[0] += n
                return v
            WB = [calloc([128, 4, 4, 128], BF16) for _ in range(2)]
            TB = [calloc([128, 4, 2, 256], F32) for _ in range(2)]
            XG = [calloc([128, 4, 256], BF16) for _ in range(2)]
            wA, wB_, wC, wD = [calloc([128, 4, 256], F32) for _ in range(4)]
            Sp = [calloc([128, 4, 256], BF16) for _ in range(2)]
            Y8 = [calloc([128, 2, 8, 128], BF16) for _ in range(2)]
            gtmp_ = [calloc([128, 512], F32) for _ in range(2)]
            wglu = calloc([128, 4, 512], BF16)
            sg = calloc([128, 4, 512], BF16)
            bglu = calloc([128, 4], F32)
            b_WB, b_TB, b_XG = [Buf("WB0"), Buf("WB1")], [Buf("TB0"), Buf("TB1")], [Buf("XG0"), Buf("XG1")]
            b_wA, b_wB, b_wC, b_wD = Buf("wA"), Buf("wB"), Buf("wC"), Buf("wD")
            b_Sp, b_Y8, b_gtmp = [Buf("Sp0"), Buf("Sp1")], [Buf("Y80"), Buf("Y81")], [Buf("gtmp0"), Buf("gtmp1")]
            b_wglu, b_sg, b_bglu, b_rho = Buf("wglu"), Buf("sg"), Buf("bglu"), Buf("rho")
            DMA("sp", [], [b_rho], semkey="dma_rho", out=rho[:], in_=RHOD[l])
            DMA("pool", [], [b_wglu], semkey="dma_wglu", out=wglu,
                in_=Dm["w_glu"][l].rearrange("(kt p) c -> p kt c", p=128))
            DMA("sp", [], [b_bglu], semkey="dma_bglu", out=bglu, in_=Dm["b_glu"][l].rearrange("(kt p) -> p kt", p=128),
                allow_slow_non_contiguous=True)
            for i in range(2):
                POOL("memset", [], [b_Sp[i]], ap=Sp[i], constant=0.0)
            for bt in range(8):
                bi = bt % 2
                g0 = 4 * bt
                DMA("sp", [], [b_WB[bi]], semkey=f"dma_WB{bi}", out=WB[bi], in_=WSSM[l, :, g0:g0 + 4, :, :])
                DMA("sp", [], [b_TB[bi]], semkey=f"dma_TB{bi}", out=TB[bi], in_=TAB[l, :, g0:g0 + 4, :, :])
                pvb = pbank[0][:].bitcast(BF16)
                for gl in range(4):
                    g = g0 + gl
                    for half in range(2):
                        slot = gl * 2 + half
                        PE("transpose", [b_U8[half], bCONST], [bP[0]], last=(slot == 7),
                           out=pvb[:, slot * 128:(slot + 1) * 128], in_=U8[:, half, g, :, :].rearrange("p j h -> p (j h)"),
                           identity=ident_bf[:])
                evac(XG[bi].rearrange("p g c -> p (g c)"), pvb, [bP[0]], [b_XG[bi]])
                for gl in range(4):
                    bk = 1 + gl // 2
                    cs_ = slice((gl % 2) * 256, (gl % 2) * 256 + 256)
                    PE("matmul", [b_WB[bi], b_XG[bi]], [bP[bk]], out=pbank[bk][:, cs_], lhsT=WB[bi][:, gl, 1, :],
                       rhs=XG[bi][:, gl, :], start=True, stop=True)
                    PE("matmul", [b_WB[bi], b_XG[bi]], [bP[bk + 2]], out=pbank[bk + 2][:, cs_], lhsT=WB[bi][:, gl, 2, :],
                       rhs=XG[bi][:, gl, :], start=True, stop=True)
                for hb in range(2):
                    gs = slice(2 * hb, 2 * hb + 2)
                    Tc_ = TB[bi][:, gs, 0, :]
                    Ts_ = TB[bi][:, gs, 1, :]
                    Sl = pbank[1 + hb][:].rearrange("p (g c) -> p g c", g=2)
                    Sw = pbank[3 + hb][:].rearrange("p (g c) -> p g c", g=2)
                    DVE("tensor_tensor", [b_TB[bi], bP[1 + hb]], [b_wA], out=wA[:, gs, :], in0=Sl, in1=Tc_, op=ALU.mult)
                    DVE("tensor_tensor", [b_TB[bi], bP[3 + hb]], [b_wB], out=wB_[:, gs, :], in0=Sw, in1=Ts_, op=ALU.mult)
                    DVE("tensor_tensor", [b_wA, b_wB], [b_wA], out=wA[:, gs, :], in0=wA[:, gs, :], in1=wB_[:, gs, :],
                        op=ALU.add)
                    DVE("tensor_tensor", [b_TB[bi], bP[3 + hb]], [b_wB], out=wB_[:, gs, :], in0=Sw, in1=Tc_, op=ALU.mult)
                    DVE("tensor_tensor", [b_TB[bi], bP[1 + hb]], [b_wC], out=wC[:, gs, :], in0=Sl, in1=Ts_, op=ALU.mult)
                    DVE("tensor_tensor", [b_wB, b_wC], [b_wB], out=wB_[:, gs, :], in0=wB_[:, gs, :], in1=wC[:, gs, :],
                        op=ALU.subtract)
                for gl in range(4):
                    g = g0 + gl
                    rb = rho[:, g:g + 1].to_broadcast([128, 256])
                    DVE("tensor_tensor_scan", [b_wA, b_rho], [b_wC], out=wC[:, gl, :], data0=rb, data1=wA[:, gl, :],
                        initial=0.0, op0=ALU.mult, op1=ALU.add)
                    DVE("tensor_tensor_scan", [b_wB, b_rho], [b_wD], out=wD[:, gl, :], data0=rb, data1=wB_[:, gl, :],
                        initial=0.0, op0=ALU.mult, op1=ALU.add)
                Tc4 = TB[bi][:, :, 0, :]
                Ts4 = TB[bi][:, :, 1, :]
                POOL("tensor_tensor", [b_wC, b_TB[bi]], [b_wA], out=wA, in0=wC, in1=Tc4, op=ALU.mult)
                POOL("tensor_tensor", [b_wD, b_TB[bi]], [b_wB], out=wB_, in0=wD, in1=Ts4, op=ALU.mult)
                DVE("tensor_tensor", [b_wA, b_wB], [b_Sp[bi]], out=Sp[bi][:, :, 1:256], in0=wA[:, :, 0:255],
                    in1=wB_[:, :, 0:255], op=ALU.subtract)
                y8 = Y8[(bt // 2) % 2]
                b_y8 = b_Y8[(bt // 2) % 2]
                c0 = (g0 % 8) * 16
                for half in range(2):
                    bk = 5 + half
                    hsl = slice(half * 128, half * 128 + 128)
                    for gl in range(4):
                        osl = slice(gl * 128, gl * 128 + 128)
                        PE("matmul", [b_XG[bi], b_WB[bi]], [bP[bk]], last=False, out=pbank[bk][:, osl],
                           lhsT=XG[bi][:, gl, hsl], rhs=WB[bi][:, gl, 0, :], start=True, stop=False)
                        PE("matmul", [b_Sp[bi], b_WB[bi]], [bP[bk]], last=(gl == 3), out=pbank[bk][:, osl],
                           lhsT=Sp[bi][:, gl, hsl], rhs=WB[bi][:, gl, 3, :], start=False, stop=True)
                    gt_ = gtmp_[half]
                    b_gt = b_gtmp[half]
                    ACT("activation", [bP[bk]], [b_gt], out=gt_, in_=pbank[bk][:], func=AF.Square)
                    DVE("tensor_scalar", [b_gt], [b_gt], out=gt_, in0=gt_, scalar1=GELU_C * 0.044715, scalar2=GELU_C,
                        op0=ALU.mult, op1=ALU.add)
                    DVE("tensor_tensor", [b_gt, bP[bk]], [b_gt], out=gt_, in0=gt_, in1=pbank[bk][:], op=ALU.mult)
                    ACT("activation", [b_gt], [b_gt], out=gt_, in_=gt_, func=AF.Sigmoid)
                    DVE("tensor_tensor", [b_gt, bP[bk]], [b_y8],
                        out=y8[:, half, :, c0:c0 + 64].rearrange("p j (g h) -> p g j h", g=4),
                        in0=gt_.rearrange("p (g j h) -> p g j h", g=4, j=8),
                        in1=pbank[bk][:].rearrange("p (g j h) -> p g j h", g=4, j=8), op=ALU.mult)
                if bt % 2 == 1:
                    ct = bt // 2
                    for half in range(2):
                        bk = 7 if half == 0 else 0
                        pvt = pbank[bk][:].bitcast(BF16)
                        for j in range(8):
                            PE("transpose", [b_y8, bCONST], [bP[bk]], last=(j == 7), out=pvt[:, j * 128:(j + 1) * 128],
                               in_=y8[:, half, j, :], identity=ident_bf[:])
                        evac(gT[:, ct, half * 1024:(half + 1) * 1024].rearrange("p (c j) -> p j c", j=8),
                             pvt.rearrange("p (j c) -> p j c", j=8), [bP[bk]], [b_gT[ct]])
            if debug and stop == "C0":
                dbg("gT", gT, [128, 4, T], BF16, b_gT)
                done = True
                break
            for c in range(4):
                cl = slice(c * 512, (c + 1) * 512)
                for ot in range(4):
                    bk = 1 + ot
                    for ct in range(4):
                        PE("matmul", [b_wglu, b_gT[ct]], [bP[bk]], last=(ct == 3), out=pbank[bk][:],
                           lhsT=wglu[:, ct, ot * 128:(ot + 1) * 128], rhs=gT[:, ct, cl], start=(ct == 0), stop=(ct == 3))
                    ACT("activation", [bP[bk], b_bglu], [b_sg], out=sg[:, ot, :], in_=pbank[bk][:], func=AF.Sigmoid,
                        bias=bglu[:, ot:ot + 1], scale=1.0)
                for ot in range(4):
                    DVE("tensor_tensor", [b_sg, b_gT[ot]], [b_gT[ot]], out=gT[:, ot, cl], in0=gT[:, ot, cl],
                        in1=sg[:, ot, :], op=ALU.mult)
            if debug and stop == "C":
                dbg("ssT", gT, [128, 4, T], BF16, b_gT)
                done = True
                break
            S.barrier()
            zero_ss()
            hTc = aview(O_U8, [128, 8, 512], BF16)
            mixtmp = aview(O_U8 + 8192, [128, 8, 512], BF16)
            WG = aview(O_QT, [128, 8, 2048], BF16)
            WO = aview(O_QT, [128, 8, 1024], BF16)
            WPA = aview(O_V, [128, 4, 1024], BF16)
            WPS = aview(O_V + 8192, [128, 4, 1024], BF16)
            od = [O_CS]

            def dalloc(shape, dt):
                n = 2 if dt == BF16 else 4
                for s_ in shape[1:]:
                    n *= s_
                v = aview(od[0], shape, dt)
                od[0] += n
                return v
            hbuf = dalloc([128, D], F32)
            hbf2 = [dalloc([128, D], BF16) for _ in range(2)]
            b_hbf2 = [Buf("hbf0"), Buf("hbf1")]
            junk = dalloc([128, D], BF16)
            stats_all(junk)
            ga = [dalloc([128, 512], F32) for _ in range(2)]
            gsm = [dalloc([128, 512], F32) for _ in range(2)]
            t1_ = [dalloc([128, 512], F32) for _ in range(2)]
            t2_ = [dalloc([128, 512], F32) for _ in range(2)]
            xtmp = [dalloc([128, 512], F32) for _ in range(2)]
            bgate = dalloc([128, 16], F32)
            b_hTc, b_mix, b_WG, b_WPA, b_WPS = Buf("hTc"), Buf("mix"), Buf("WG"), Buf("WPA"), Buf("WPS")
            b_h, b_hbf, b_junk, b_bgate = Buf("h"), Buf("hbf"), Buf("junk"), Buf("bgate")
            b_ga, b_gsm = [Buf("ga0"), Buf("ga1")], [Buf("gs0"), Buf("gs1")]
            b_t1, b_t2, b_xtmp = [Buf("t10"), Buf("t11")], [Buf("t20"), Buf("t21")], [Buf("xt0"), Buf("xt1")]
            for g in range(4):
                DMA("pool", [], [b_WG], semkey="dma_WG", out=WG[:, 2 * g:2 * g + 2, :],
                    in_=Dm["w_in"][l, 256 * g:256 * g + 256, 2056:4104].rearrange("(kt p) c -> p kt c", p=128))
            DMA("pool", [], [b_WPA], semkey="dma_WPA", out=WPA,
                in_=Dm["w_proj_att"][l].rearrange("(kt p) c -> p kt c", p=128))
            DMA("pool", [], [b_WPS], semkey="dma_WPS", out=WPS,
                in_=Dm["w_proj_ssm"][l].rearrange("(kt p) c -> p kt c", p=128))
            DMA("sp", [], [b_bgate], semkey="dma_bgate", out=bgate, in_=Dm["b_gate"][l].rearrange("(ft p) -> p ft", p=128),
                allow_slow_non_contiguous=True)
            for c in range(4):
                cl = slice(c * 512, (c + 1) * 512)
                for t4 in range(4):
                    tt = 4 * c + t4
                    modulated_h_tile(tt, hbuf, b_h, hbf2[tt % 2], b_hbf2[tt % 2], junk, b_junk)
                    transpose_tile(hbf2[tt % 2], b_hbf2[tt % 2], hTc, b_hTc, t4 * 128, 6 + (t4 % 2))
                for dtile in range(8):
                    i2 = dtile % 2
                    for (gi, gbuf, b_g) in ((0, ga[i2], b_ga[i2]), (1, gsm[i2], b_gsm[i2])):
                        bk = gi + 4 * i2
                        col0 = gi * 1024 + dtile * 128
                        for kt in range(8):
                            PE("matmul", [b_WG, b_hTc], [bP[bk]], last=(kt == 7), out=pbank[bk][:],
                               lhsT=WG[:, kt, col0:col0 + 128], rhs=hTc[:, kt, :], start=(kt == 0), stop=(kt == 7))
                        ACT("activation", [bP[bk], b_bgate], [b_g], out=gbuf, in_=pbank[bk][:], func=AF.Sigmoid,
                            bias=bgate[:, gi * 8 + dtile:gi * 8 + dtile + 1], scale=1.0)
                    bka, bks = 2 + 4 * i2, 3 + 4 * i2
                    for ct in range(4):
                        PE("matmul", [b_WPA, b_attT[ct]], [bP[bka]], last=(ct == 3), out=pbank[bka][:],
                           lhsT=WPA[:, ct, dtile * 128:(dtile + 1) * 128], rhs=attT[:, ct, cl], start=(ct == 0),
                           stop=(ct == 3))
                    for ct in range(4):
                        PE("matmul", [b_WPS, b_gT[ct]], [bP[bks]], last=(ct == 3), out=pbank[bks][:],
                           lhsT=WPS[:, ct, dtile * 128:(dtile + 1) * 128], rhs=gT[:, ct, cl], start=(ct == 0),
                           stop=(ct == 3))
                    DVE("tensor_tensor", [b_ga[i2], bP[bka]], [b_t1[i2]], out=t1_[i2], in0=pbank[bka][:], in1=ga[i2],
                        op=ALU.mult)
                    DVE("tensor_tensor", [b_gsm[i2], bP[bks]], [b_t2[i2]], out=t2_[i2], in0=pbank[bks][:], in1=gsm[i2],
                        op=ALU.mult)
                    POOL("tensor_tensor", [b_t1[i2], b_t2[i2]], [b_mix], out=mixtmp[:, dtile, :], in0=t1_[i2],
                         in1=t2_[i2], op=ALU.add)
                ACT("copy", [b_mix], b_attT, out=attT[:, :, cl], in_=mixtmp[:, 0:4, :])
                POOL("tensor_copy", [b_mix], b_gT, out=gT[:, :, cl], in_=mixtmp[:, 4:8, :])
            for g in range(4):
                DMA("pool", [], [b_WG], semkey="dma_WG", out=WO[:, 2 * g:2 * g + 2, :],
                    in_=Dm["w_out"][l, 256 * g:256 * g + 256, :].rearrange("(kt p) c -> p kt c", p=128))
            it = 0
            for tt in range(NT):
                tsl = slice(tt * 128, (tt + 1) * 128)
                for half in range(2):
                    bk = it % 4
                    xt = xtmp[it % 2]
                    b_xt = b_xtmp[it % 2]
                    it += 1
                    for kt in range(8):
                        src = attT[:, kt, tsl] if kt < 4 else gT[:, kt - 4, tsl]
                        b_src = b_attT[kt] if kt < 4 else b_gT[kt - 4]
                        PE("matmul", [b_WG, b_src], [bP[bk]], last=(kt == 7), out=pbank[bk][:], lhsT=src,
                           rhs=WO[:, kt, half * 512:(half + 1) * 512], start=(kt == 0), stop=(kt == 7))
                    hsl = slice(half * 512, (half + 1) * 512)
                    DVE("tensor_tensor", [bP[bk], bADA], [b_xt], out=xt, in0=pbank[bk][:], in1=ADA[:, 2, hsl], op=ALU.mult)
                    POOL("tensor_tensor", [b_xt, bX[tt]], [bX[tt]], out=X[:, tt, hsl], in0=X[:, tt, hsl], in1=xt, op=ALU.add)
            if debug and stop == "D":
                dbg("X1", X[:], [128, NT, D], F32, bX)
                done = True
                break
            S.barrier()
            stats_all(aview(0, [128, D], BF16), early=True)
            compute_ada(s, l, 1, 32768)
            S.barrier()
            moe = (l % 2 == 1)
            if moe and sparse:
                sparse_moe(s, l)
                if debug and stop in ("R", f"E{l}"):
                    if stop != "R":
                        dbg("X2", X[:], [128, NT, D], F32, bX)
                    done = True
                    break
                S.barrier()
                continue
            h2T = aview(0, [128, 8, T], BF16)
            actT = aview(32768, [128, 6, T], BF16)
            oe = [57344]

            def ealloc(shape, dt):
                n = 2 if dt == BF16 else 4
                for s_ in shape[1:]:
                    n *= s_
                v = aview(oe[0], shape, dt)
                oe[0] += n
                return v
            W1S = [ealloc([128, 8, 128], BF16) for _ in range(3)]
            W3S = [ealloc([128, 8, 128], BF16) for _ in range(3)]
            W2G = [ealloc([128, 6, 1024], BF16) for _ in range(2)]
            hbuf = ealloc([128, D], F32)
            hbufb = ealloc([128, D], F32)
            b_hb = Buf("hb")
            hbf2 = [ealloc([128, D], BF16) for _ in range(2)]
            b_hbf2 = [Buf("hbf0"), Buf("hbf1")]
            junk = ealloc([128, D], BF16)
            stats_all(junk)
            sil = [ealloc([128, 512], F32) for _ in range(2)]
            xtmp = [ealloc([128, 512], F32) for _ in range(2)]
            WR = ealloc([128, 8, 8], BF16)
            logits = ealloc([128, NT, 8], F32)
            gates = ealloc([128, NT, 8], F32)
            eq = ealloc([128, NT, 8], F32)
            l2 = ealloc([128, NT, 8], F32)
            m1 = ealloc([128, NT], F32)
            m2 = ealloc([128, NT], F32)
            brt = ealloc([128, 8], F32)
            b_h2T, b_act = Buf("h2T"), [Buf(f"act{i}") for i in range(6)]
            b_W1S, b_W3S = [Buf(f"W1S{i}") for i in range(3)], [Buf(f"W3S{i}") for i in range(3)]
            b_W2G = [Buf("W2G0"), Buf("W2G1")]
            b_h, b_hbf, b_junk = Buf("h"), Buf("hbf"), Buf("junk")
            b_sil, b_xtmp = [Buf("sil0"), Buf("sil1")], [Buf("xt0"), Buf("xt1")]
            b_WR, b_rt = Buf("WR"), Buf("router")
            for tt in range(NT):
                modulated_h_tile(tt, (hbuf, hbufb)[tt % 2], (b_h, b_hb)[tt % 2], hbf2[tt % 2], b_hbf2[tt % 2], junk, b_junk)
                transpose_tile(hbf2[tt % 2], b_hbf2[tt % 2], h2T, b_h2T, tt * 128, 6 + (tt % 2))
            if moe:
                m = l // 2
                DMA("pool", [], [b_WR], semkey="dma_WR", out=WR, in_=Dm["w_router"][m].rearrange("(kt p) e -> p kt e", p=128))
                DMA("sp", [], [b_rt], semkey="dma_brt", out=brt, in_=Dm["b_router"][m].partition_broadcast(128))
                pv = pbank[0][:].rearrange("p (a b) -> p a b", a=NT)[:, :, 0:8]
                for tt in range(NT):
                    for kt in range(8):
                        PE("matmul", [b_WR, b_h2T], [bP[0]], last=(kt == 7 and tt == NT - 1), out=pv[:, tt, :],
                           lhsT=h2T[:, kt, tt * 128:(tt + 1) * 128], rhs=WR[:, kt, :], start=(kt == 0), stop=(kt == 7))
                s3 = [128, NT, 8]
                rR, wR = [b_rt], [b_rt]
                DVE("tensor_tensor", [bP[0], b_rt], wR, out=logits, in0=pv, in1=brt.unsqueeze(1).to_broadcast(s3), op=ALU.add)
                DVE("tensor_reduce", rR, wR, out=m1, in_=logits, axis=AX.X, op=ALU.max)
                DVE("tensor_tensor", rR, wR, out=eq, in0=logits, in1=m1.unsqueeze(2).to_broadcast(s3), op=ALU.is_equal)
                DVE("scalar_tensor_tensor", rR, wR, out=l2, in0=eq, scalar=-1.0e30, in1=logits, op0=ALU.mult, op1=ALU.add)
                DVE("tensor_reduce", rR, wR, out=m2, in_=l2, axis=AX.X, op=ALU.max)
                DVE("tensor_tensor", rR, wR, out=eq, in0=logits, in1=m2.unsqueeze(2).to_broadcast(s3), op=ALU.is_ge)
                DVE("tensor_tensor", rR, wR, out=l2, in0=logits, in1=m1.unsqueeze(2).to_broadcast(s3), op=ALU.subtract)
                ACT("activation", rR, wR, out=l2, in_=l2, func=AF.Exp)
                DVE("tensor_tensor", rR, wR, out=l2, in0=l2, in1=eq, op=ALU.mult)
                DVE("tensor_reduce", rR, wR, out=m2, in_=l2, axis=AX.X, op=ALU.add)
                DVE("reciprocal", rR, wR, out=m2, in_=m2)
                DVE("tensor_tensor", rR, wR, out=gates, in0=l2, in1=m2.unsqueeze(2).to_broadcast(s3), op=ALU.mult)
                if debug and stop == "R":
                    dbg("gates", gates, [128, NT, 8], F32, [b_rt])
                    done = True
                    break
            n_exp = NE if moe else 1
            slab = 0
            gcount = 0
            pe_it = 0
            ev_it = 0
            for e in range(n_exp):
                if moe:
                    w1d, w3d, w2d = Dm["w1_moe"][0, e], Dm["w3_moe"][0, e], Dm["w2_moe"][0, e]
                else:
                    w1d, w3d, w2d = Dm["w1_dense"][l // 2], Dm["w3_dense"][l // 2], Dm["w2_dense"][l // 2]
                for (f0, nf) in FGROUPS:
                    gb = gcount % 2
                    gcount += 1
                    DMA("pool", [], [b_W2G[gb]], semkey=f"dma_W2G{gb}", out=W2G[gb][:, 0:nf, :],
                        in_=w2d[f0 * 128:(f0 + nf) * 128, :].rearrange("(ft p) d -> p ft d", p=128))
                    for fl in range(nf):
                        ft = f0 + fl
                        si = slab % 3
                        slab += 1
                        DMA("pool", [], [b_W1S[si]], semkey=f"dma_W1S{si}", out=W1S[si],
                            in_=w1d[:, ft * 128:(ft + 1) * 128].rearrange("(kt p) c -> p kt c", p=128))
                        DMA("pool", [], [b_W3S[si]], semkey=f"dma_W3S{si}", out=W3S[si],
                            in_=w3d[:, ft * 128:(ft + 1) * 128].rearrange("(kt p) c -> p kt c", p=128))
                        for c in range(4):
                            cl = slice(c * 512, (c + 1) * 512)
                            pa, pb_ = (pe_it % 2) * 2, (pe_it % 2) * 2 + 1
                            sl_, b_sl = sil[pe_it % 2], b_sil[pe_it % 2]
                            pe_it += 1
                            for kt in range(8):
                                PE("matmul", [b_W1S[si], b_h2T], [bP[pa]], last=(kt == 7), out=pbank[pa][:],
                                   lhsT=W1S[si][:, kt, :], rhs=h2T[:, kt, cl], start=(kt == 0), stop=(kt == 7))
                            for kt in range(8):
                                PE("matmul", [b_W3S[si], b_h2T], [bP[pb_]], last=(kt == 7), out=pbank[pb_][:],
                                   lhsT=W3S[si][:, kt, :], rhs=h2T[:, kt, cl], start=(kt == 0), stop=(kt == 7))
                            ACT("activation", [bP[pa]], [b_sl], out=sl_, in_=pbank[pa][:], func=AF.Silu)
                            DVE("tensor_tensor", [b_sl, bP[pb_]], [b_act[fl]], out=actT[:, fl, cl], in0=sl_,
                                in1=pbank[pb_][:], op=ALU.mult)
                    for tt in range(NT):
                        tsl = slice(tt * 128, (tt + 1) * 128)
                        for half in range(2):
                            bk = 4 + (ev_it % 4)
                            xt, b_xt = xtmp[ev_it % 2], b_xtmp[ev_it % 2]
                            ev_it += 1
                            hsl = slice(half * 512, (half + 1) * 512)
                            for fl in range(nf):
                                PE("matmul", [b_act[fl], b_W2G[gb]], [bP[bk]], last=(fl == nf - 1), out=pbank[bk][:],
                                   lhsT=actT[:, fl, tsl], rhs=W2G[gb][:, fl, hsl], start=(fl == 0), stop=(fl == nf - 1))
                            if moe:
                                DVE("scalar_tensor_tensor", [bP[bk], b_rt, bADA], [b_xt], out=xt, in0=pbank[bk][:],
                                    scalar=gates[:, tt, e:e + 1], in1=ADA[:, 2, hsl], op0=ALU.mult, op1=ALU.mult)
                            else:
                                DVE("tensor_tensor", [bP[bk], bADA], [b_xt], out=xt, in0=pbank[bk][:], in1=ADA[:, 2, hsl],
                                    op=ALU.mult)
                            DVE("tensor_tensor", [b_xt, bX[tt]], [bX[tt]], out=X[:, tt, hsl], in0=X[:, tt, hsl], in1=xt,
                                op=ALU.add)
            if debug and stop == f"E{l}":
                dbg("X2", X[:], [128, NT, D], F32, bX)
                done = True
                break
            S.barrier()
        if done:
            break
        if n_layers == DEPTH:
            zero_ss()
            gfin = aview(0, [128, D], F32)
            junk = aview(4096, [128, D], BF16)
            ob_ = [aview(8192 + i * 4096, [128, D], F32) for i in range(2)]
            b_gf, b_junk, b_ob = Buf("gfin"), Buf("junk"), [Buf("ob0"), Buf("ob1")]
            DMA("sp", [], [b_gf], semkey="dma_gfin", out=gfin, in_=Dm["g_final"].partition_broadcast(128))
            stats_all(junk)
            for tt in range(NT):
                norm_tile(tt, ob_[tt % 2], b_ob[tt % 2], junk, b_junk, gfin, [b_gf])
                DMA("sp", [b_ob[tt % 2]], [], semkey=f"dma_out{tt % 2}", out=out_d[s, tt * 128:(tt + 1) * 128, :],
                    in_=ob_[tt % 2])
            S.barrier()

    S.barrier()
    S.run()
    st.close()
    return nc, dbg_out


_CACHE = {}


def kernel(**inputs):
    if "nc" not in _CACHE:
        _CACHE["nc"] = build()[0]
    nc = _CACHE["nc"]
    in_maps = []
    for i in range(NCORES):
        m = {}
        for k in IN_SPECS:
            a = np.asarray(inputs[k], dtype=np.float32)
            if k in ("x", "c"):
                a = a[2 * i:2 * i + 2]
            m[k] = np.ascontiguousarray(a)
        in_maps.append(m)
    res = run_bass_kernel_spmd(nc, in_maps, core_ids=list(range(NCORES)))
    return np.concatenate([np.asarray(r["out"]) for r in res.results], axis=0).astype(np.float32)
```
